# Optimizing a Trainium2 kernel written in Bass

```python
import math
import jax, jax.numpy as jnp
from jax import lax
import numpy as np

D_MODEL = 1024
BATCH = 2
SEQ = 16384
DEPTH = 1

PLE_DIM = 256
EPS = 1e-6

ATTN_HEADS = 8
ATTN_HEAD_DIM = 64
ATTN_WIDTH = ATTN_HEADS * ATTN_HEAD_DIM
DILATED_PAIRS = ((128, 1), (512, 4), (2048, 16))
ATTN_BLOCK = 128

MLSTM_HEADS = 4
MLSTM_HEAD_DIM = 128
MLSTM_WIDTH = MLSTM_HEADS * MLSTM_HEAD_DIM
MLSTM_CHUNK = 64
CONV_WIDTH = 4

PEER_HEADS = 8
PEER_N_KEYS = 128
PEER_N_EXPERTS = PEER_N_KEYS * PEER_N_KEYS
PEER_QUERY_DIM = 256
PEER_HALF_DIM = PEER_QUERY_DIM // 2
PEER_TOPK = 16
PEER_TOKEN_BLOCK = 128

N_IN = 3 * ATTN_WIDTH + 4 * MLSTM_WIDTH + 2 * MLSTM_HEADS + 2 * D_MODEL
IN_SPLITS = (
    ATTN_WIDTH,
    2 * ATTN_WIDTH,
    3 * ATTN_WIDTH,
    3 * ATTN_WIDTH + 2 * MLSTM_WIDTH,
    3 * ATTN_WIDTH + 3 * MLSTM_WIDTH,
    3 * ATTN_WIDTH + 4 * MLSTM_WIDTH,
    3 * ATTN_WIDTH + 4 * MLSTM_WIDTH + 2 * MLSTM_HEADS,
)

kernel_name = "hybrid_dilated_mlstm_peer_block"


def _rmsnorm(x, g):
    xf = x.astype(jnp.float32)
    y = xf * lax.rsqrt(jnp.mean(xf * xf, axis=-1, keepdims=True) + EPS) * g.astype(jnp.float32)
    return y.astype(x.dtype)


def _dilated_window_attention(q, k, v, window, dilation):
    b, s, h, e = q.shape
    w_sub = window // dilation
    sub_len = s // dilation
    nb = -(-sub_len // ATTN_BLOCK)
    lp = nb * ATTN_BLOCK
    scale = 1.0 / math.sqrt(e)

    def to_sub(t):
        t = t.reshape(b, sub_len, dilation, h, e).transpose(0, 2, 3, 1, 4)
        return jnp.pad(t, ((0, 0), (0, 0), (0, 0), (0, lp - sub_len), (0, 0)))

    def with_prev(t):
        tb = jnp.pad(t, ((0, 0), (0, 0), (0, 0), (ATTN_BLOCK, 0), (0, 0)))
        tb = tb.reshape(b, dilation, h, nb + 1, ATTN_BLOCK, e)
        return jnp.concatenate([tb[:, :, :, :-1], tb[:, :, :, 1:]], axis=4)

    qb = to_sub(q).reshape(b, dilation, h, nb, ATTN_BLOCK, e)
    kb = with_prev(to_sub(k))
    vb = with_prev(to_sub(v))

    scores = jnp.einsum('bdhnqe,bdhnke->bdhnqk', qb, kb).astype(jnp.float32) * scale
    blk = jnp.arange(nb)[:, None, None] * ATTN_BLOCK
    q_pos = blk + jnp.arange(ATTN_BLOCK)[None, :, None]
    k_pos = blk - ATTN_BLOCK + jnp.arange(2 * ATTN_BLOCK)[None, None, :]
    dist = q_pos - k_pos
    mask = (dist >= 0) & (dist <= w_sub) & (k_pos >= 0)
    scores = jnp.where(mask, scores, -jnp.inf)
    m = jnp.max(scores, axis=-1, keepdims=True)
    pr = jnp.exp(scores - m)
    denom = jnp.sum(pr, axis=-1, keepdims=True)
    out = jnp.einsum('bdhnqk,bdhnke->bdhnqe', (pr / denom).astype(v.dtype), vb).astype(jnp.float32)
    lse = (m + jnp.log(denom))[..., 0]

    out = out.reshape(b, dilation, h, lp, e)[:, :, :, :sub_len]
    out = out.transpose(0, 3, 1, 2, 4).reshape(b, s, h, e)
    lse = lse.reshape(b, dilation, h, lp)[:, :, :, :sub_len]
    lse = lse.transpose(0, 3, 1, 2).reshape(b, s, h)
    return out, lse


def _causal_depthwise_conv(x, w):
    c = x.shape[-1]
    return lax.conv_general_dilated(
        x, w.astype(x.dtype)[:, None, :], window_strides=(1,),
        padding=((CONV_WIDTH - 1, 0),), dimension_numbers=('NWC', 'WIO', 'NWC'),
        feature_group_count=c)


def _mlstm_chunkwise(q, k, v, i_pre, log_f):
    b, s, h, e = q.shape
    nc = s // MLSTM_CHUNK

    def chunks(t):
        t = t.astype(jnp.float32).reshape(b, nc, MLSTM_CHUNK, h, *t.shape[3:])
        return jnp.moveaxis(t, (1, 3), (0, 2))

    qc, kc, vc = chunks(q), chunks(k), chunks(v)
    ic, fc = chunks(i_pre), chunks(log_f)
    causal = jnp.tril(jnp.ones((MLSTM_CHUNK, MLSTM_CHUNK), dtype=bool))

    def step(carry, inp):
        c_st, n_st, m_st = carry
        qj, kj, vj, ij, fj = inp
        bcum = jnp.cumsum(fj, axis=-1)
        g = bcum[..., -1]
        d_intra = bcum[..., :, None] - bcum[..., None, :] + ij[..., None, :]
        d_intra = jnp.where(causal, d_intra, -jnp.inf)
        d_inter = bcum + m_st[..., None]
        m_t = jnp.maximum(d_inter, jnp.max(d_intra, axis=-1))
        w_intra = jnp.exp(d_intra - m_t[..., None])
        w_inter = jnp.exp(d_inter - m_t)
        qk = jnp.einsum('bhte,bhse->bhts', qj, kj) * w_intra
        num = (w_inter[..., None] * jnp.einsum('bhte,bhef->bhtf', qj, c_st)
               + jnp.einsum('bhts,bhsf->bhtf', qk, vj))
        den = w_inter * jnp.einsum('bhte,bhe->bht', qj, n_st) + jnp.sum(qk, axis=-1)
        h_out = num / jnp.maximum(jnp.abs(den), jnp.exp(-m_t))[..., None]
        d_state = g[..., None] - bcum + ij
        m_new = jnp.maximum(g + m_st, jnp.max(d_state, axis=-1))
        w_old = jnp.exp(g + m_st - m_new)
        w_s = jnp.exp(d_state - m_new[..., None])
        c_new = w_old[..., None, None] * c_st + jnp.einsum('bhs,bhse,bhsf->bhef', w_s, kj, vj)
        n_new = w_old[..., None] * n_st + jnp.einsum('bhs,bhse->bhe', w_s, kj)
        return (c_new, n_new, m_new), h_out

    init = (jnp.zeros((b, h, e, e), jnp.float32), jnp.zeros((b, h, e), jnp.float32),
            jnp.zeros((b, h), jnp.float32))
    _, hs = lax.scan(step, init, (qc, kc, vc, ic, fc))
    return jnp.moveaxis(hs, (0, 2), (1, 3)).reshape(b, s, h, e)


def _peer(h, w_query, keys1, keys2, expert_u, expert_v):
    b, s, d = h.shape
    q = (h @ w_query).reshape(b, s, PEER_HEADS, 2, PEER_HALF_DIM)
    s1 = jnp.einsum('bshe,hne->bshn', q[..., 0, :], keys1).astype(jnp.float32)
    s2 = jnp.einsum('bshe,hne->bshn', q[..., 1, :], keys2).astype(jnp.float32)
    v1, i1 = lax.top_k(s1, PEER_TOPK)
    v2, i2 = lax.top_k(s2, PEER_TOPK)
    cand_score = (v1[..., :, None] + v2[..., None, :]).reshape(b, s, PEER_HEADS, PEER_TOPK * PEER_TOPK)
    cand_idx = (i1[..., :, None] * PEER_N_KEYS + i2[..., None, :]).reshape(b, s, PEER_HEADS, PEER_TOPK * PEER_TOPK)
    top_score, top_pos = lax.top_k(cand_score, PEER_TOPK)
    idx = jnp.take_along_axis(cand_idx, top_pos, axis=-1)
    gates = jax.nn.softmax(top_score, axis=-1).astype(h.dtype)

    n_blk = (b * s) // PEER_TOKEN_BLOCK
    xt = h.reshape(n_blk, PEER_TOKEN_BLOCK, d)
    it = idx.reshape(n_blk, PEER_TOKEN_BLOCK, PEER_HEADS, PEER_TOPK)
    gt = gates.reshape(n_blk, PEER_TOKEN_BLOCK, PEER_HEADS, PEER_TOPK)

    def block(args):
        xb, ib, gb = args
        u_sel = jnp.take(expert_u, ib, axis=0)
        act = jax.nn.gelu(jnp.einsum('thkd,td->thk', u_sel, xb), approximate=False)
        v_sel = jnp.take(expert_v, ib, axis=0)
        return jnp.einsum('thk,thkd->td', gb * act, v_sel)

    y = lax.map(block, (xt, it, gt))
    return y.reshape(b, s, d)


def setup_inputs(seed: int = 0) -> dict:
    key = jax.random.key(seed)
    ks = jax.random.split(key, 24)
    f32 = jnp.float32

    def nrm(k, shape, scale):
        return jax.random.normal(k, shape, f32) * scale

    def gain(k, shape):
        return 1.0 + 0.05 * jax.random.normal(k, shape, f32)

    i_bias = 0.1 * jax.random.normal(ks[3], (DEPTH, MLSTM_HEADS), f32)
    f_bias = jnp.linspace(3.0, 6.0, MLSTM_HEADS, dtype=f32)[None, :] + 0.01 * jax.random.normal(ks[4], (DEPTH, MLSTM_HEADS), f32)
    return {
        "x": nrm(ks[0], (BATCH, SEQ, D_MODEL), 1.0),
        "p": nrm(ks[1], (DEPTH, BATCH, SEQ, PLE_DIM), 1.0),
        "norm_mix": gain(ks[2], (DEPTH, D_MODEL)),
        "w_in": nrm(ks[5], (DEPTH, D_MODEL, N_IN), D_MODEL ** -0.5),
        "conv_qk": nrm(ks[6], (DEPTH, CONV_WIDTH, 2 * MLSTM_WIDTH), CONV_WIDTH ** -0.5),
        "gate_bias": jnp.concatenate([i_bias, f_bias], axis=-1),
        "mlstm_norm": gain(ks[7], (DEPTH, MLSTM_WIDTH)),
        "w_up_attn": nrm(ks[8], (DEPTH, ATTN_WIDTH, D_MODEL), ATTN_WIDTH ** -0.5),
        "w_up_mlstm": nrm(ks[9], (DEPTH, MLSTM_WIDTH, D_MODEL), MLSTM_WIDTH ** -0.5),
        "w_out": nrm(ks[10], (DEPTH, D_MODEL, D_MODEL), D_MODEL ** -0.5),
        "norm_ffn": gain(ks[11], (DEPTH, D_MODEL)),
        "w_query": nrm(ks[12], (DEPTH, D_MODEL, PEER_HEADS * PEER_QUERY_DIM), D_MODEL ** -0.5),
        "keys1": nrm(ks[13], (DEPTH, PEER_HEADS, PEER_N_KEYS, PEER_HALF_DIM), PEER_HALF_DIM ** -0.5),
        "keys2": nrm(ks[14], (DEPTH, PEER_HEADS, PEER_N_KEYS, PEER_HALF_DIM), PEER_HALF_DIM ** -0.5),
        "expert_u": nrm(ks[15], (DEPTH, PEER_N_EXPERTS, D_MODEL), D_MODEL ** -0.5),
        "expert_v": nrm(ks[16], (DEPTH, PEER_N_EXPERTS, D_MODEL), PEER_HEADS ** -0.5),
        "norm_ple": gain(ks[17], (DEPTH, D_MODEL)),
        "w_ple_gate": nrm(ks[18], (DEPTH, D_MODEL, D_MODEL), D_MODEL ** -0.5),
        "w_ple": nrm(ks[19], (DEPTH, PLE_DIM, D_MODEL), PLE_DIM ** -0.5),
        "norm_final": gain(ks[20], (D_MODEL,)),
    }


def reference(x, p, norm_mix, w_in, conv_qk, gate_bias, mlstm_norm, w_up_attn, w_up_mlstm,
              w_out, norm_ffn, w_query, keys1, keys2, expert_u, expert_v, norm_ple,
              w_ple_gate, w_ple, norm_final):
    b, s, _ = x.shape
    for layer in range(DEPTH):
        h = _rmsnorm(x, norm_mix[layer])
        proj = h @ w_in[layer]
        q_a, k_a, v_a, qk_m, v_m, o_m, if_m, gate_pre = jnp.split(proj, IN_SPLITS, axis=-1)

        qa = q_a.reshape(b, s, ATTN_HEADS, ATTN_HEAD_DIM)
        ka = k_a.reshape(b, s, ATTN_HEADS, ATTN_HEAD_DIM)
        va = v_a.reshape(b, s, ATTN_HEADS, ATTN_HEAD_DIM)
        outs, lses = [], []
        for window, dilation in DILATED_PAIRS:
            o_g, lse_g = _dilated_window_attention(qa, ka, va, window, dilation)
            outs.append(o_g)
            lses.append(lse_g)
        mix_w = jax.nn.softmax(jnp.stack(lses, axis=0), axis=0)
        y_a = jnp.sum(mix_w[..., None] * jnp.stack(outs, axis=0), axis=0)
        y_a = y_a.reshape(b, s, ATTN_WIDTH).astype(x.dtype)

        qk_c = jax.nn.silu(_causal_depthwise_conv(qk_m, conv_qk[layer]))
        q_m, k_m = jnp.split(qk_c, 2, axis=-1)
        q_m = q_m.reshape(b, s, MLSTM_HEADS, MLSTM_HEAD_DIM)
        k_m = k_m.reshape(b, s, MLSTM_HEADS, MLSTM_HEAD_DIM) * (MLSTM_HEAD_DIM ** -0.5)
        vm = v_m.reshape(b, s, MLSTM_HEADS, MLSTM_HEAD_DIM)
        gif = if_m.astype(jnp.float32) + gate_bias[layer].astype(jnp.float32)
        i_pre, f_pre = jnp.split(gif, 2, axis=-1)
        h_m = _mlstm_chunkwise(q_m, k_m, vm, i_pre, jax.nn.log_sigmoid(f_pre))
        h_m = h_m * lax.rsqrt(jnp.mean(h_m * h_m, axis=-1, keepdims=True) + EPS)
        y_m = (h_m.reshape(b, s, MLSTM_WIDTH) * mlstm_norm[layer].astype(jnp.float32)).astype(x.dtype)
        y_m = y_m * jax.nn.sigmoid(o_m)

        g_a, g_m = jnp.split(gate_pre, 2, axis=-1)
        merged = (jax.nn.sigmoid(g_a) * (y_a @ w_up_attn[layer])
                  + jax.nn.sigmoid(g_m) * (y_m @ w_up_mlstm[layer]))
        x = x + merged @ w_out[layer]

        x = x + _peer(_rmsnorm(x, norm_ffn[layer]), w_query[layer], keys1[layer], keys2[layer],
                      expert_u[layer], expert_v[layer])

        ple_gate = jax.nn.sigmoid(_rmsnorm(x, norm_ple[layer]) @ w_ple_gate[layer])
        x = x + ple_gate * (p[layer] @ w_ple[layer])
    return _rmsnorm(x, norm_final)
```

```python
from contextlib import ExitStack
import numpy as np
import concourse.bass as bass
import concourse.mybir as mybir
from concourse.bass_utils import run_bass_kernel_spmd

F32 = mybir.dt.float32
BF16 = mybir.dt.bfloat16
U32 = mybir.dt.uint32
I32 = mybir.dt.int32
ALU = mybir.AluOpType
AF = mybir.ActivationFunctionType
AX = mybir.AxisListType

SEM_MAX = 30000
NTOK = 4096
NHIST = 12288
HALO = 2048


class Buf:
    __slots__ = ("name", "writes", "reads", "dsem", "dcount")

    def __init__(self, name):
        self.name = name
        self.writes = {}
        self.reads = {}
        self.dsem = None
        self.dcount = 0


class FW:
    def __init__(self, nc, same_engine_sync=True):
        self.nc = nc
        self.es = ExitStack()
        self.same = same_engine_sync
        self.eng = {"pe": nc.tensor, "act": nc.scalar, "dve": nc.vector,
                    "pool": nc.gpsimd, "sp": nc.sync}
        self.sems = {}
        self.cnt = {}
        self.ekey = {}
        self.nep = 0
        for k in self.eng:
            self.ekey[k] = k + "#0"
            self.sems[self.ekey[k]] = self.es.enter_context(nc.semaphore("s_" + k + "_0"))
            self.cnt[k] = 0
        self.known = {k: {} for k in self.eng}
        self.nd = 0
        self.ninstr = 0
        self.dmax = {}

    def sbuf(self, name, shape, dt, es=None):
        return (es or self.es).enter_context(self.nc.sbuf_tensor(name, list(shape), dt))

    def psum(self, name, shape, dt):
        return self.es.enter_context(self.nc.psum_tensor(name, list(shape), dt))

    def buf(self, name):
        return Buf(name)

    def _dsem(self, b):
        if b.dsem is None or b.dcount + 16 > SEM_MAX:
            b.dcount = 0
            key = "d%d" % self.nd
            self.nd += 1
            self.sems[key] = self.es.enter_context(self.nc.semaphore(key))
            b.dsem = key
        return b.dsem

    def _waits(self, e, reads, writes, extra=None, skip_self=False):
        need = dict(extra) if extra else {}
        for b in reads:
            for s, v in b.writes.items():
                if need.get(s, 0) < v:
                    need[s] = v
        for b in writes:
            for s, v in b.writes.items():
                if need.get(s, 0) < v:
                    need[s] = v
            for s, v in b.reads.items():
                if need.get(s, 0) < v:
                    need[s] = v
        kn = self.known[e]
        eng = self.eng[e]
        for s, v in need.items():
            if s.split("#")[0] == e and (e == "pe" or not self.same or skip_self):
                continue
            if kn.get(s, 0) >= v:
                continue
            eng.wait_ge(self.sems[s], v)
            kn[s] = v
            self.ninstr += 1

    def op(self, e, ins, reads=(), writes=(), skip_self=False):
        self._waits(e, reads, writes, skip_self=skip_self)
        i = ins(self.eng[e])
        if self.cnt[e] >= SEM_MAX:
            self.nep += 1
            self.ekey[e] = "%s#%d" % (e, self.nep)
            self.sems[self.ekey[e]] = self.es.enter_context(
                self.nc.semaphore("s_%s_%d" % (e, self.nep)))
            self.cnt[e] = 0
        ek = self.ekey[e]
        self.cnt[e] += 1
        c = self.cnt[e]
        i.then_inc(self.sems[ek], 1)
        self.ninstr += 1
        for b in writes:
            b.writes = {ek: c}
            b.reads = {}
        for b in reads:
            if b.reads.get(ek, 0) < c:
                b.reads[ek] = c
        return i

    def dma(self, q, ins, side, reads=(), writes=()):
        self._waits(q, reads, writes)
        i = ins(self.eng[q])
        s = self._dsem(side)
        side.dcount += 16
        c = side.dcount
        i.then_inc(self.sems[s], 16)
        self.dmax[s] = c
        self.ninstr += 1
        for b in writes:
            b.writes = {s: c}
            b.reads = {}
        for b in reads:
            if b.reads.get(s, 0) < c:
                b.reads[s] = c
        return i

    def barrier(self):
        tot = {self.ekey[k]: self.cnt[k] for k in self.eng if self.cnt[k] > 0}
        tot.update(self.dmax)
        for e in self.eng:
            self._waits(e, (), (), extra=tot)

    def wait_all(self, e, bufs):
        self._waits(e, bufs, bufs)


C_QA, C_KA, C_VA = 0, 512, 1024
C_QKM, C_VM, C_OM, C_IF, C_GA, C_GM = 1536, 2560, 3072, 3584, 3592, 4616
N_IN = 5640


def build(stage=99):
    nc = bass.Bass("TRN2", target_bir_lowering=False)

    def din(name, shape, dt=F32):
        return nc.dram_tensor(name, list(shape), dt, kind="ExternalInput").ap()

    xh = din("xh", [NHIST + NTOK, 1024])
    pin = din("p", [NTOK, 256])
    hv_d = din("hv", [128, 1])
    w_in = din("w_in", [1024, N_IN])
    norm_mix = din("norm_mix", [1024])
    conv_qk = din("conv_qk", [4, 1024])
    gate_bias = din("gate_bias", [1, 8])
    mlstm_norm = din("mlstm_norm", [1, 512])
    w_up_attn = din("w_up_attn", [512, 1024])
    w_up_mlstm = din("w_up_mlstm", [512, 1024])
    w_out = din("w_out", [1024, 1024])
    norm_ffn = din("norm_ffn", [1, 1024])
    w_query = din("w_query", [1024, 2048])
    keys1 = din("keys1", [8, 128, 128])
    keys2 = din("keys2", [8, 128, 128])
    expert_u = din("expert_u", [16384, 1024])
    expert_v = din("expert_v", [16384, 1024])
    norm_ple = din("norm_ple", [1024])
    w_ple_gate = din("w_ple_gate", [1024, 1024])
    w_ple = din("w_ple", [256, 1024])
    norm_final = din("norm_final", [1, 1024])
    out_d = nc.dram_tensor("out", [NTOK, 1024], F32, kind="ExternalOutput").ap()
    scrV = nc.dram_tensor("scrV", [HALO + NTOK, 8, 128], BF16, kind="Internal").ap()
    scrYa = nc.dram_tensor("scrYa", [128, 4, NTOK], BF16, kind=("ExternalOutput" if stage == 1 else "Internal")).ap()
    scrUV = nc.dram_tensor("scrUV", [16384, 2048], BF16, kind="Internal").ap()
    scrX1 = nc.dram_tensor("scrX1", [NTOK, 1024], F32, kind=("ExternalOutput" if stage == 2 else "Internal")).ap()

    f = FW(nc)
    op, dma = f.op, f.dma

    cst = f.sbuf("cst", [128, 8], F32); B_cst = Buf("cst")
    identf = f.sbuf("identf", [128, 128], F32); B_identf = Buf("identf")
    identb = f.sbuf("identb", [128, 128], BF16); B_identb = Buf("identb")
    maskU = f.sbuf("maskU", [128, 128], F32); B_maskU = Buf("maskU")
    maskL = f.sbuf("maskL", [128, 128], F32); B_maskL = Buf("maskL")
    onesf = f.sbuf("onesf", [128, 128], F32); B_onesf = Buf("onesf")
    hv = f.sbuf("hvt", [128, 1], F32); B_hv = Buf("hv")
    EPS, ONE = cst[:, 0:1], cst[:, 1:2]

    op("pool", lambda e: e.memset(cst[:, 0:1], 1e-6), writes=[B_cst])
    op("pool", lambda e: e.memset(cst[:, 1:2], 1.0), writes=[B_cst])
    op("pool", lambda e: e.memset(cst[:, 2:8], 0.0), writes=[B_cst])
    op("pool", lambda e: e.memset(onesf[:], 1.0), writes=[B_onesf])
    op("pool", lambda e: e.memset(identf[:], 1.0), writes=[B_identf])
    op("pool", lambda e: e.affine_select(out=identf[:], in_=identf[:], pattern=[[-1, 128]],
                                         compare_op=ALU.is_equal, fill=0.0, base=0, channel_multiplier=1),
       reads=[B_identf], writes=[B_identf])
    op("dve", lambda e: e.tensor_copy(out=identb[:], in_=identf[:]), reads=[B_identf], writes=[B_identb])
    op("pool", lambda e: e.memset(maskU[:], 1.0), writes=[B_maskU])
    op("pool", lambda e: e.affine_select(out=maskU[:], in_=maskU[:], pattern=[[1, 128]],
                                         compare_op=ALU.is_ge, fill=0.0, base=0, channel_multiplier=-1),
       reads=[B_maskU], writes=[B_maskU])
    op("pool", lambda e: e.memset(maskL[:], 1.0), writes=[B_maskL])
    op("pool", lambda e: e.affine_select(out=maskL[:], in_=maskL[:], pattern=[[-1, 128]],
                                         compare_op=ALU.is_ge, fill=0.0, base=0, channel_multiplier=1),
       reads=[B_maskL], writes=[B_maskL])
    dma("sp", lambda e: e.dma_start(out=hv[:], in_=hv_d), B_hv, writes=[B_hv])

    PS = [f.psum("ps%d" % i, [128, 512], F32) for i in range(7)]
    B_PS = [Buf("ps%d" % i) for i in range(7)]
    PT = f.psum("pt", [128, 1024], BF16); B_PT = Buf("pt")

    NSET = 2
    n_jk = f.sbuf("n_jk", [128, 1024], BF16)
    nrm = []
    for i in range(NSET):
        nrm.append(dict(
            ss=f.sbuf("n_ss%d" % i, [128, 1], F32), B_ss=Buf("n_ss"),
            rs=f.sbuf("n_rs%d" % i, [128, 1], F32), B_rs=Buf("n_rs"),
            xs=f.sbuf("n_xs%d" % i, [128, 1024], BF16), B_xs=Buf("n_xs")))
    nrm_i = [0]

    def _run(g):
        try:
            while True:
                next(g)
        except StopIteration as e:
            return e.value

    def rms_g(xt_ap, B_x, width=1024):
        s = nrm[nrm_i[0] % len(nrm)]; nrm_i[0] += 1
        op("act", lambda e: e.activation(out=n_jk[:, 0:width], in_=xt_ap, func=AF.Square, accum_out=s["ss"][:]),
           reads=[B_x], writes=[s["B_ss"]])
        op("act", lambda e: e.activation(out=s["rs"][:], in_=s["ss"][:], func=AF.Sqrt, scale=1.0 / width, bias=EPS),
           reads=[s["B_ss"], B_cst], writes=[s["B_rs"]])
        yield
        op("dve", lambda e: e.reciprocal(out=s["rs"][:], in_=s["rs"][:]), reads=[s["B_rs"]], writes=[s["B_rs"]])
        return s

    def norm_T_g(xt_ap, B_x, dst_ap, B_dst, pt_ap=None, B_pt_=None):
        if pt_ap is None:
            pt_ap, B_pt_ = PT[:], B_PT
        s = yield from rms_g(xt_ap, B_x)
        op("act", lambda e: e.activation(out=s["xs"][:], in_=xt_ap, func=AF.Copy, scale=s["rs"][:, 0:1]),
           reads=[B_x, s["B_rs"]], writes=[s["B_xs"]])
        for c in range(8):
            op("pe", lambda e: e.transpose(out=pt_ap[:, c * 128:(c + 1) * 128], in_=s["xs"][:, c * 128:(c + 1) * 128],
                                           identity=identb[:]), reads=[s["B_xs"], B_identb], writes=[B_pt_])
        yield
        op("dve", lambda e: e.tensor_copy(out=dst_ap, in_=pt_ap.rearrange("p (c t) -> p c t", c=8)),
           reads=[B_pt_], writes=[B_dst])
        return s

    def rms(xt_ap, B_x, width=1024):
        return _run(rms_g(xt_ap, B_x, width))

    def norm_T(xt_ap, B_x, dst_ap, B_dst):
        return _run(norm_T_g(xt_ap, B_x, dst_ap, B_dst))

    stg_i = [0]
    stg_cur = dict(t=None, B=None)

    def set_stg(aps):
        stg_cur["t"] = aps
        stg_cur["B"] = [Buf("stgA"), Buf("stgB")]

    def load_w(dst, B_dst, src, r0, nchunk, c0, ncol, gain=None, B_gain=None, dcol0=0):
        stg, B_stg = stg_cur["t"], stg_cur["B"]
        for kc in range(nchunk):
            for cc in range(0, ncol, 512):
                w = min(512, ncol - cc)
                i = stg_i[0] % 2; stg_i[0] += 1
                dma("sp", lambda e: e.dma_start(out=stg[i][:, 0:w], in_=src[r0 + kc * 128: r0 + (kc + 1) * 128, c0 + cc: c0 + cc + w]),
                    B_stg[i], writes=[B_stg[i]])
                if gain is None:
                    op("act", lambda e: e.activation(out=dst[:, kc, dcol0 + cc: dcol0 + cc + w], in_=stg[i][:, 0:w], func=AF.Copy),
                       reads=[B_stg[i]], writes=[B_dst])
                else:
                    op("act", lambda e: e.activation(out=dst[:, kc, dcol0 + cc: dcol0 + cc + w], in_=stg[i][:, 0:w], func=AF.Copy,
                                                     scale=gain[:, kc:kc + 1]),
                       reads=[B_stg[i], B_gain], writes=[B_dst])

    gmix = f.sbuf("gmix", [128, 8], F32); B_gmix = Buf("gmix")
    gple = f.sbuf("gple", [128, 8], F32); B_gple = Buf("gple")
    gffn = f.sbuf("gffn", [128, 8], F32); B_gffn = Buf("gffn")
    dma("sp", lambda e: e.dma_start(out=gmix[:], in_=norm_mix.rearrange("(c p) -> p c", p=128), allow_slow_non_contiguous=True),
        B_gmix, writes=[B_gmix])
    dma("sp", lambda e: e.dma_start(out=gple[:], in_=norm_ple.rearrange("(c p) -> p c", p=128), allow_slow_non_contiguous=True),
        B_gple, writes=[B_gple])
    dma("sp", lambda e: e.dma_start(out=gffn[:], in_=norm_ffn[0].rearrange("(c p) -> p c", p=128), allow_slow_non_contiguous=True),
        B_gffn, writes=[B_gffn])

    xts = [f.sbuf("xt%d" % i, [128, 1024], F32) for i in range(4)]
    B_xts = [Buf("xt%d" % i) for i in range(4)]
    hT = f.sbuf("hT", [128, 8, 512], BF16); B_hT = Buf("hT")

    def pass_attention():
        es = ExitStack()
        wA = f.sbuf("wA", [128, 8, 1536], BF16, es); B_wA = Buf("wA")
        accN = f.sbuf("accN", [128, 2, 2048], F32, es); B_accN = Buf("accN")
        set_stg([accN[:, 0, 0:512], accN[:, 1, 0:512]])
        load_w(wA, B_wA, w_in, 0, 8, 0, 1536, gmix, B_gmix)
        f.barrier()
        hT2 = f.sbuf("hT2", [128, 8, 512], BF16, es)
        hTb = [hT, hT2]; B_hTb = [Buf("hTa"), Buf("hTb")]
        kT = f.sbuf("kT", [128, 4, HALO + NTOK], BF16, es)
        B_kT = [Buf("kT%d" % i) for i in range(3)]
        qp = f.sbuf("qp", [128, 8, 2048], BF16, es); B_qp = Buf("qp")
        op("pool", lambda e: e.memset(qp[:], 0.0), writes=[B_qp])
        vpad = [f.sbuf("vpad%d" % i, [128, 8, 128], BF16, es) for i in range(2)]
        B_vpad = [Buf("vpad0"), Buf("vpad1")]
        for i in range(2):
            op("pool", lambda e: e.memset(vpad[i][:], 0.0), writes=[B_vpad[i]])
        B_scrV = [Buf("scrV%d" % i) for i in range(3)]
        onespad = f.sbuf("onespad", [128, 2, 128], BF16, es); B_op = Buf("onespad")
        op("pool", lambda e: e.memset(onespad[:], 0.0), writes=[B_op])
        op("pool", lambda e: e.memset(onespad[:, 0, 0:64], 1.0), writes=[B_op])
        op("pool", lambda e: e.memset(onespad[:, 1, 64:128], 1.0), writes=[B_op])
        amN = f.sbuf("amN", [128, 2, 2, 128], F32, es); B_amN = Buf("amN")
        amH = f.sbuf("amH", [128, 2, 2, 128], F32, es); B_amH = Buf("amH")
        for hh in range(2):
            op("dve", lambda e: e.tensor_copy(out=amN[:, hh, 0, :], in_=maskL[:]), reads=[B_maskL], writes=[B_amN])
            op("dve", lambda e: e.tensor_copy(out=amN[:, hh, 1, :], in_=maskU[:]), reads=[B_maskU], writes=[B_amN])
            op("dve", lambda e: e.tensor_scalar(out=amH[:, hh, 0, :], in0=maskL[:], scalar1=hv[:, 0:1], scalar2=None, op0=ALU.mult),
               reads=[B_maskL, B_hv], writes=[B_amH])
            op("dve", lambda e: e.tensor_copy(out=amH[:, hh, 1, :], in_=maskU[:]), reads=[B_maskU], writes=[B_amH])
        accD = f.sbuf("accD", [128, 2, 2048], F32, es); B_accD = Buf("accD")
        yb = f.sbuf("yb", [128, 2, 2048], BF16, es); B_yb = Buf("yb")
        Et = [f.sbuf("Et%d" % i, [128, 512], F32, es) for i in range(2)]
        B_Et = [Buf("Et0"), Buf("Et1")]
        Pb = [f.sbuf("Pb%d" % i, [128, 512], BF16, es) for i in range(2)]
        B_Pb = [Buf("Pb0"), Buf("Pb1")]
        vt = [f.sbuf("vt%d" % i, [128, 4, 128], BF16, es) for i in range(3)]
        B_vt = [Buf("vt%d" % i) for i in range(3)]
        B_scrYa = Buf("scrYa")
        cnt = dict(e=0, v=0)

        def attention(QB):
            uW = QB * 2048
            for half in range(2):
                op("pool", lambda e: e.memset(accN[:], 0.0), writes=[B_accN])
                op("pool", lambda e: e.memset(accD[:], 0.0), writes=[B_accD])
                units = []
                for d in (1, 4, 16):
                    span = 128 * d
                    m0 = (uW + 2048) // span
                    nm = 2048 // span
                    for r in range(d):
                        for m in range(m0, m0 + nm):
                            for hp2 in range(2):
                                units.append((d, r, m, hp2, m == m0))
                st = dict(iprev=None, icur=None)
                def load_v(d, r, m):
                    span = 128 * d
                    i = cnt["v"] % 3; cnt["v"] += 1
                    u0 = span * m + r
                    blk = u0 // 2048
                    dma("sp", lambda e: e.dma_start(out=vt[i][:], in_=scrV[u0: u0 + span - d + 1: d, 4 * half: 4 * half + 4, :]),
                        B_vt[i], reads=[B_scrV[blk]], writes=[B_vt[i]])
                    return i

                def emitS(idx):
                    d, r, m, hp2, first = units[idx]
                    span = 128 * d
                    hp = 2 * half + hp2
                    sb = idx % 2
                    sps, B_sps = PS[2 + sb], B_PS[2 + sb]
                    uq = span * m + r - (uW + 2048)
                    qsl = slice(uq, uq + span - d + 1, d)
                    for hh in range(2):
                        h = 2 * hp + hh
                        for blk in range(2):
                            uk = span * (m - 1 + blk) + r
                            kb = uk // 2048
                            o = (hh * 2 + blk) * 128
                            op("pe", lambda e: e.matmul(sps[:, o:o + 128], lhsT=kT[:, hp, uk: uk + span - d + 1: d],
                                                        rhs=qp[:, h, qsl], start=True, stop=True),
                               reads=[B_kT[kb], B_qp], writes=[B_sps])

                def emitEM(idx):
                    d, r, m, hp2, first = units[idx]
                    span = 128 * d
                    sb = idx % 2
                    sps, B_sps = PS[2 + sb], B_PS[2 + sb]
                    prev_halo = (span * (m - 1)) < 2048
                    op("act", lambda e: e.activation(out=Et[sb][:], in_=sps[:], func=AF.Exp, scale=0.125),
                       reads=[B_sps], writes=[B_Et[sb]])
                    am, B_am = (amH, B_amH) if prev_halo else (amN, B_amN)
                    op("dve", lambda e: e.tensor_tensor(out=Pb[sb][:], in0=Et[sb][:],
                                                        in1=am[:].rearrange("p a b q -> p (a b q)"), op=ALU.mult),
                       reads=[B_Et[sb], B_am], writes=[B_Pb[sb]])

                def emitPV(idx):
                    d, r, m, hp2, first = units[idx]
                    span = 128 * d
                    sb = idx % 2
                    if (idx // 2) % 2 == 0:
                        nb, db, B_nb, B_db = PS[4], PS[5], B_PS[4], B_PS[5]
                    else:
                        nb, db, B_nb, B_db = PS[0], PS[1], B_PS[0], B_PS[1]
                    if hp2 == 0:
                        if first:
                            st["iprev"] = load_v(d, r, m - 1)
                        st["icur"] = load_v(d, r, m)
                    vsl = (st["iprev"], st["icur"])
                    k = 0
                    for hh in range(2):
                        for blk in range(2):
                            o = (hh * 2 + blk) * 128
                            op("pe", lambda e: e.matmul(nb[:, hp2 * 128:(hp2 + 1) * 128],
                                                        lhsT=vt[vsl[blk]][:, 2 * hp2 + hh, :],
                                                        rhs=Pb[sb][:, o:o + 128], start=(k == 0), stop=(k == 3)),
                               reads=[B_vt[vsl[blk]], B_Pb[sb]], writes=[B_nb])
                            k += 1
                    k = 0
                    for hh in range(2):
                        for blk in range(2):
                            o = (hh * 2 + blk) * 128
                            op("pe", lambda e: e.matmul(db[:, hp2 * 128:(hp2 + 1) * 128],
                                                        lhsT=onespad[:, hh, :],
                                                        rhs=Pb[sb][:, o:o + 128], start=(k == 0), stop=(k == 3)),
                               reads=[B_op, B_Pb[sb]], writes=[B_db])
                            k += 1
                    if hp2 == 1:
                        uq = span * m + r - (uW + 2048)
                        qsl = slice(uq, uq + span - d + 1, d)
                        op("dve", lambda e: e.tensor_tensor(out=accN[:, :, qsl], in0=nb[:, 0:256].rearrange("p (a q) -> p a q", a=2),
                                                            in1=accN[:, :, qsl], op=ALU.add),
                           reads=[B_nb, B_accN], writes=[B_accN])
                        op("dve", lambda e: e.tensor_tensor(out=accD[:, :, qsl], in0=db[:, 0:256].rearrange("p (a q) -> p a q", a=2),
                                                            in1=accD[:, :, qsl], op=ALU.add),
                           reads=[B_db, B_accD], writes=[B_accD])
                        st["iprev"] = st["icur"]

                emitS(0)
                for idx in range(len(units)):
                    emitEM(idx)
                    if idx + 1 < len(units):
                        emitS(idx + 1)
                    emitPV(idx)
                op("dve", lambda e: e.reciprocal(out=accD[:], in_=accD[:]), reads=[B_accD], writes=[B_accD])
                op("dve", lambda e: e.tensor_tensor(out=yb[:], in0=accN[:], in1=accD[:], op=ALU.mult),
                   reads=[B_accN, B_accD], writes=[B_yb])
                dma("sp", lambda e: e.dma_start(out=scrYa[:, 2 * half: 2 * half + 2, QB * 2048:(QB + 1) * 2048], in_=yb[:]),
                    B_yb, reads=[B_yb], writes=[])
                B_scrYa.writes[B_yb.dsem] = B_yb.dcount

        def prefetchA(g):
            j0 = NHIST - HALO + g * 512
            for ti in range(4):
                dma("sp", lambda e: e.dma_start(out=xts[ti][:], in_=xh[j0 + ti * 128: j0 + (ti + 1) * 128, :]),
                    B_xts[ti], writes=[B_xts[ti]])
            for ti in range(4):
                norm_T(xts[ti][:], B_xts[ti], hTb[g % 2][:, :, ti * 128:(ti + 1) * 128], B_hTb[g % 2])

        prefetchA(0)
        for g in range(12):
            u0 = g * 512
            hTc, B_hTc = hTb[g % 2], B_hTb[g % 2]
            kb = u0 // 2048
            for c in range(4):
                ps, B_ps = PS[c % 2], B_PS[c % 2]
                for kc in range(8):
                    op("pe", lambda e: e.matmul(ps[:], lhsT=wA[:, kc, C_KA + c * 128: C_KA + (c + 1) * 128], rhs=hTc[:, kc, :],
                                                start=(kc == 0), stop=(kc == 7)), reads=[B_wA, B_hTc], writes=[B_ps])
                op("act", lambda e: e.activation(out=kT[:, c, u0:u0 + 512], in_=ps[:], func=AF.Copy),
                   reads=[B_ps], writes=[B_kT[kb]])
            if g + 1 < 12:
                prefetchA(g + 1)
            if g >= 4:
                uq0 = ((g - 4) % 4) * 512
                for c in range(4):
                    ps, B_ps = PS[c % 2], B_PS[c % 2]
                    for kc in range(8):
                        op("pe", lambda e: e.matmul(ps[:], lhsT=wA[:, kc, C_QA + c * 128: C_QA + (c + 1) * 128], rhs=hTc[:, kc, :],
                                                    start=(kc == 0), stop=(kc == 7)), reads=[B_wA, B_hTc], writes=[B_ps])
                    op("dve", lambda e: e.tensor_copy(out=qp[0:64, 2 * c, uq0:uq0 + 512], in_=ps[0:64, :]),
                       reads=[B_ps], writes=[B_qp])
                    op("act", lambda e: e.activation(out=qp[64:128, 2 * c + 1, uq0:uq0 + 512], in_=ps[64:128, :], func=AF.Copy),
                       reads=[B_ps], writes=[B_qp])
            for ti in range(4):
                ps, B_ps = PS[ti % 2], B_PS[ti % 2]
                vi = ti % 2
                for kc in range(8):
                    op("pe", lambda e: e.matmul(ps[:], lhsT=hTc[:, kc, ti * 128:(ti + 1) * 128], rhs=wA[:, kc, C_VA:C_VA + 512],
                                                start=(kc == 0), stop=(kc == 7)), reads=[B_wA, B_hTc], writes=[B_ps])
                pv = ps[:].rearrange("p (h e) -> p h e", e=64)
                op("dve", lambda e: e.tensor_copy(out=vpad[vi][:, 0:8:2, 0:64], in_=pv[:, 0:8:2, :]),
                   reads=[B_ps], writes=[B_vpad[vi]])
                op("act", lambda e: e.activation(out=vpad[vi][:, 1:8:2, 64:128], in_=pv[:, 1:8:2, :], func=AF.Copy),
                   reads=[B_ps], writes=[B_vpad[vi]])
                dma("sp", lambda e: e.dma_start(out=scrV[u0 + ti * 128: u0 + (ti + 1) * 128], in_=vpad[vi][:]),
                    B_vpad[vi], reads=[B_vpad[vi]], writes=[B_scrV[kb]])
            if g == 7:
                attention(0)
            if g == 11:
                attention(1)
        f.barrier()
        es.close()
        return B_scrYa

    B_uv = Buf("scrUV")

    def prepass_tables():
        es = ExitStack()
        st = [f.sbuf("pst%d" % i, [128, 8, 1024], F32, es) for i in range(2)]
        B_st = [Buf("pst0"), Buf("pst1")]
        cv = [f.sbuf("pcv%d" % i, [128, 8, 1024], BF16, es) for i in range(2)]
        B_cv = [Buf("pcv0"), Buf("pcv1")]
        dv = scrUV.rearrange("(p r) d -> p r d", p=128)
        jobs = [(tb, c) for tb in range(2) for c in range(16)]

        def load(k):
            tb, c = jobs[k]
            sv = (expert_u if tb == 0 else expert_v).rearrange("(p r) d -> p r d", p=128)
            dma("sp", lambda e: e.dma_start(out=st[k % 2][:], in_=sv[:, 8 * c:8 * c + 8, :]), B_st[k % 2], writes=[B_st[k % 2]])
        load(0)
        for k, (tb, c) in enumerate(jobs):
            i = k % 2
            if k + 1 < len(jobs):
                load(k + 1)
            if k % 2 == 0:
                op("act", lambda e: e.activation(out=cv[i][:], in_=st[i][:], func=AF.Copy), reads=[B_st[i]], writes=[B_cv[i]])
            else:
                op("dve", lambda e: e.tensor_copy(out=cv[i][:], in_=st[i][:]), reads=[B_st[i]], writes=[B_cv[i]])
            dma("sp", lambda e: e.dma_start(out=dv[:, 8 * c:8 * c + 8, tb * 1024:(tb + 1) * 1024], in_=cv[i][:]),
                B_cv[i], reads=[B_cv[i]], writes=[])
            B_uv.writes[B_cv[i].dsem] = B_cv[i].dcount
        f.barrier()
        es.close()

    prepass_tables()

    B_scrYa = pass_attention()

    if stage == 1:
        f.barrier()
        return nc, f


    M_QKM, M_VM, M_OM, M_IF, M_GA, M_GM = 0, 1024, 1536, 2048, 2056, 3080

    def pass_mlstm():
        es = ExitStack()
        wM = f.sbuf("wM", [128, 8, 4104], BF16, es); B_wM = Buf("wM")
        scr4 = f.sbuf("scr4", [128, 1024], F32, es); B_scr4 = Buf("scr4")
        set_stg([scr4[:, 0:512], scr4[:, 512:1024]])
        load_w(wM, B_wM, w_in, 0, 8, 1536, 4104, gmix, B_gmix)
        wUA = f.sbuf("wUA", [128, 4, 1024], BF16, es); B_wUA = Buf("wUA")
        wUM = f.sbuf("wUM", [128, 4, 1024], BF16, es); B_wUM = Buf("wUM")
        wOut = f.sbuf("wOut", [128, 8, 1024], BF16, es); B_wOut = Buf("wOut")
        load_w(wUA, B_wUA, w_up_attn, 0, 4, 0, 1024)
        load_w(wUM, B_wUM, w_up_mlstm, 0, 4, 0, 1024)
        load_w(wOut, B_wOut, w_out, 0, 8, 0, 1024)
        f.barrier()
        hT2 = f.sbuf("hT2m", [128, 8, 512], BF16, es)
        hTb = [hT, hT2]; B_hTb = [Buf("hTa"), Buf("hTb")]
        mnorm = f.sbuf("mnorm", [128, 512], F32, es); B_mnorm = Buf("mnorm")
        dma("sp", lambda e: e.dma_start(out=mnorm[:], in_=mlstm_norm.to_broadcast([128, 512])), B_mnorm, writes=[B_mnorm])
        gbias = f.sbuf("gbias", [128, 8], F32, es); B_gbias = Buf("gbias")
        dma("sp", lambda e: e.dma_start(out=gbias[:], in_=gate_bias.to_broadcast([128, 8])), B_gbias, writes=[B_gbias])
        cw = f.sbuf("cw", [128, 8, 4], F32, es); B_cw = Buf("cw")
        for k in range(4):
            dma("sp", lambda e: e.dma_start(out=cw[:, :, k], in_=conv_qk[k].rearrange("(c p) -> p c", p=128),
                                            allow_slow_non_contiguous=True), B_cw, writes=[B_cw])
        gif = f.sbuf("gif", [128, 4, 8], F32, es); B_gif = Buf("gif")
        e1 = f.sbuf("e1", [128, 4, 4], F32, es); B_e1 = Buf("e1")
        nsp = f.sbuf("nsp", [128, 4, 4], F32, es); B_nsp = Buf("nsp")
        eFs = f.sbuf("eFs", [128, 4, 4], F32, es); B_eFs = Buf("eFs")
        sp = f.sbuf("spl", [128, 4, 4], F32, es); B_sp = Buf("sp")
        rhsF = scr4[:, 0:512].rearrange("p (a b) -> p a b", a=4); B_rhsF = B_scr4
        rhsK = scr4[:, 512:1024].rearrange("p (a b) -> p a b", a=4); B_rhsK = B_scr4
        tmpI = f.sbuf("tmpI", [128, 4, 128], F32, es); B_tmpI = Buf("tmpI")
        bq_g = f.sbuf("bq_g", [128, 4, 512], F32, es); B_bq = Buf("bq")
        bk_g = f.sbuf("bk_g", [128, 4, 512], F32, es); B_bk = Buf("bk")
        pre = [f.sbuf("pre%d" % i, [128, 515], F32, es) for i in range(2)]
        B_pre = [Buf("pre0"), Buf("pre1")]
        cacc = [f.sbuf("cacc%d" % i, [128, 512], F32, es) for i in range(2)]
        B_cacc = [Buf("cacc0"), Buf("cacc1")]
        carry = f.sbuf("carry", [128, 8, 3], F32, es); B_carry = [Buf("carry%d" % i) for i in range(8)]
        op("pool", lambda e: e.memset(carry[:], 0.0), writes=B_carry)
        qTp = f.sbuf("qTp", [128, 4, 512], BF16, es); B_qTp = Buf("qTp")
        kTp = f.sbuf("kTp", [128, 4, 512], BF16, es); B_kTp = Buf("kTp")
        Vaug = [f.sbuf("Vaug%d" % i, [128, 4, 129], BF16, es) for i in range(2)]
        B_Vaug = [Buf("Vaug0"), Buf("Vaug1")]
        for i in range(2):
            op("pool", lambda e: e.memset(Vaug[i][:], 1.0), writes=[B_Vaug[i]])
        kpt = f.sbuf("kpt", [128, 4, 128], BF16, es); B_kpt = Buf("kpt")
        St = f.sbuf("St", [128, 4, 128], BF16, es); B_St = Buf("St")
        Cs = f.sbuf("Cs", [128, 4, 129], F32, es); B_Cs = Buf("Cs")
        Cb = f.sbuf("Cb", [128, 4, 129], BF16, es); B_Cb = Buf("Cb")
        op("pool", lambda e: e.memset(Cs[:], 0.0), writes=[B_Cs])
        op("pool", lambda e: e.memset(Cb[:], 0.0), writes=[B_Cb])
        dn = f.sbuf("dn", [128, 4], F32, es); B_dn = Buf("dn")
        ssh = f.sbuf("ssh", [128, 4], F32, es); B_ssh = Buf("ssh")
        rn = f.sbuf("rn", [128, 4], F32, es); B_rn = Buf("rn")
        hm = f.sbuf("hm", [128, 4, 128], F32, es); B_hm = Buf("hm")
        sqh = tmpI; B_sqh = B_tmpI
        ymb = f.sbuf("ymb", [128, 512], BF16, es); B_ymb = Buf("ymb")
        ymT = f.sbuf("ymT", [128, 4, 512], BF16, es); B_ymT = Buf("ymT")
        yaT = f.sbuf("yaT", [128, 4, 512], BF16, es); B_yaT = Buf("yaT")
        sigA = f.sbuf("sigA", [128, 512], F32, es); B_sigA = Buf("sigA")
        so = sigA; B_so = B_sigA
        sigM = f.sbuf("sigM", [128, 512], F32, es); B_sigM = Buf("sigM")
        mT = f.sbuf("mT", [128, 8, 512], BF16, es); B_mT = Buf("mT")
        x1t = scr4; B_x1t = B_scr4
        B_scrX1 = Buf("scrX1")
        maskU_bc = maskU[:].unsqueeze(1).to_broadcast([128, 4, 128])
        ident_bc = identf[:].unsqueeze(1).to_broadcast([128, 4, 128])

        def prefetchM(g):
            j0 = g * 512
            for ti in range(4):
                dma("sp", lambda e: e.dma_start(out=xts[ti][:], in_=xh[j0 + ti * 128: j0 + (ti + 1) * 128, :]),
                    B_xts[ti], writes=[B_xts[ti]])
            for ti in range(4):
                norm_T(xts[ti][:], B_xts[ti], hTb[g % 2][:, :, ti * 128:(ti + 1) * 128], B_hTb[g % 2])

        prefetchM(0)
        for g in range(32):
            own = g >= 24
            do_q = g >= 23
            j0 = g * 512
            hTc, B_hTc = hTb[g % 2], B_hTb[g % 2]
            for ti in range(4):
                ps, B_ps = PS[ti % 2], B_PS[ti % 2]
                for kc in range(8):
                    op("pe", lambda e: e.matmul(ps[:, 0:8], lhsT=hTc[:, kc, ti * 128:(ti + 1) * 128], rhs=wM[:, kc, M_IF:M_IF + 8],
                                                start=(kc == 0), stop=(kc == 7)), reads=[B_hTc, B_wM], writes=[B_ps])
                op("dve", lambda e: e.tensor_tensor(out=gif[:, ti, :], in0=ps[:, 0:8], in1=gbias[:], op=ALU.add),
                   reads=[B_ps, B_gbias], writes=[B_gif])
            op("act", lambda e: e.activation(out=e1[:], in_=gif[:, :, 4:8], func=AF.Exp, scale=-1.0),
               reads=[B_gif], writes=[B_e1])
            op("act", lambda e: e.activation(out=sp[:], in_=e1[:], func=AF.Ln, bias=ONE),
               reads=[B_e1, B_cst], writes=[B_sp])
            if not own:
                op("dve", lambda e: e.tensor_scalar(out=nsp[:], in0=sp[:], scalar1=-1.0, scalar2=None, op0=ALU.mult),
                   reads=[B_sp], writes=[B_nsp])
            for ti in range(4):
                sp_bc = sp[:, ti, :].unsqueeze(2).to_broadcast([128, 4, 128])
                i_bc = gif[:, ti, 0:4].unsqueeze(2).to_broadcast([128, 4, 128])
                if own:
                    op("dve", lambda e: e.scalar_tensor_tensor(out=rhsF, in0=maskU_bc, scalar=-1.0, in1=sp_bc,
                                                               op0=ALU.mult, op1=ALU.mult),
                       reads=[B_maskU, B_sp], writes=[B_rhsF])
                op("dve", lambda e: e.tensor_tensor(out=rhsK, in0=maskU_bc, in1=sp_bc, op=ALU.mult),
                   reads=[B_maskU, B_sp], writes=[B_rhsK])
                op("pool", lambda e: e.tensor_tensor(out=tmpI[:], in0=ident_bc, in1=i_bc, op=ALU.mult),
                   reads=[B_identf, B_gif], writes=[B_tmpI])
                op("dve", lambda e: e.tensor_tensor(out=rhsK, in0=rhsK, in1=tmpI[:], op=ALU.add),
                   reads=[B_rhsK, B_tmpI], writes=[B_rhsK])
                if own:
                    op("pe", lambda e: e.matmul(PS[0][:], lhsT=onesf[:], rhs=scr4[:, 0:512], start=True, stop=True),
                       reads=[B_onesf, B_rhsF], writes=[B_PS[0]])
                else:
                    op("pe", lambda e: e.matmul(PS[0][:, 0:4], lhsT=onesf[:], rhs=nsp[:, ti, :], start=True, stop=True),
                       reads=[B_onesf, B_nsp], writes=[B_PS[0]])
                op("pe", lambda e: e.matmul(PS[1][:], lhsT=onesf[:], rhs=scr4[:, 512:1024], start=True, stop=True),
                   reads=[B_onesf, B_rhsK], writes=[B_PS[1]])
                if own:
                    op("act", lambda e: e.activation(out=bq_g[:, :, ti * 128:(ti + 1) * 128],
                                                     in_=PS[0][:].rearrange("p (a b) -> p a b", a=4), func=AF.Exp),
                       reads=[B_PS[0]], writes=[B_bq])
                else:
                    op("act", lambda e: e.activation(out=eFs[:, ti, :], in_=PS[0][:, 0:4], func=AF.Exp),
                       reads=[B_PS[0]], writes=[B_eFs])
                op("act", lambda e: e.activation(out=bk_g[:, :, ti * 128:(ti + 1) * 128],
                                                 in_=PS[1][:].rearrange("p (a b) -> p a b", a=4), func=AF.Exp),
                   reads=[B_PS[1]], writes=[B_bk])
            for c in (range(8) if do_q else range(4, 8)):
                i2 = c % 2
                ps, B_ps = PS[i2], B_PS[i2]
                for kc in range(8):
                    op("pe", lambda e: e.matmul(ps[:], lhsT=wM[:, kc, M_QKM + c * 128: M_QKM + (c + 1) * 128], rhs=hTc[:, kc, :],
                                                start=(kc == 0), stop=(kc == 7)), reads=[B_hTc, B_wM], writes=[B_ps])
                op("pool", lambda e: e.tensor_copy(out=pre[i2][:, 0:3], in_=carry[:, c, :]), reads=[B_carry[c]], writes=[B_pre[i2]])
                op("act", lambda e: e.activation(out=pre[i2][:, 3:515], in_=ps[:], func=AF.Copy), reads=[B_ps], writes=[B_pre[i2]])
                op("pool", lambda e: e.tensor_copy(out=carry[:, c, :], in_=pre[i2][:, 512:515]), reads=[B_pre[i2]], writes=[B_carry[c]])
                op("dve", lambda e: e.tensor_scalar(out=cacc[i2][:], in0=pre[i2][:, 3:515], scalar1=cw[:, c, 3:4], scalar2=None, op0=ALU.mult),
                   reads=[B_pre[i2], B_cw], writes=[B_cacc[i2]])
                for k in (2, 1, 0):
                    op("dve", lambda e: e.scalar_tensor_tensor(out=cacc[i2][:], in0=pre[i2][:, k:k + 512], scalar=cw[:, c, k:k + 1],
                                                               in1=cacc[i2][:], op0=ALU.mult, op1=ALU.add),
                       reads=[B_pre[i2], B_cw, B_cacc[i2]], writes=[B_cacc[i2]])
                op("act", lambda e: e.activation(out=cacc[i2][:], in_=cacc[i2][:], func=AF.Silu), reads=[B_cacc[i2]], writes=[B_cacc[i2]])
                if c < 4:
                    if own:
                        op("dve", lambda e: e.tensor_tensor(out=qTp[:, c, :], in0=cacc[i2][:], in1=bq_g[:, c, :], op=ALU.mult),
                           reads=[B_cacc[i2], B_bq], writes=[B_qTp])
                else:
                    op("dve", lambda e: e.scalar_tensor_tensor(out=kTp[:, c - 4, :], in0=cacc[i2][:], scalar=128.0 ** -0.5,
                                                               in1=bk_g[:, c - 4, :], op0=ALU.mult, op1=ALU.mult),
                       reads=[B_cacc[i2], B_bk], writes=[B_kTp])
            if g + 1 < 32:
                prefetchM(g + 1)
            for ti in range(4):
                tsl = slice(ti * 128, (ti + 1) * 128)
                ps, B_ps = PS[ti % 2], B_PS[ti % 2]
                Va, B_Va = Vaug[ti % 2], B_Vaug[ti % 2]
                for kc in range(8):
                    op("pe", lambda e: e.matmul(ps[:], lhsT=hTc[:, kc, tsl], rhs=wM[:, kc, M_VM:M_VM + 512],
                                                start=(kc == 0), stop=(kc == 7)), reads=[B_hTc, B_wM], writes=[B_ps])
                op("act", lambda e: e.activation(out=Va[:, :, 0:128], in_=ps[:].rearrange("p (a b) -> p a b", a=4), func=AF.Copy),
                   reads=[B_ps], writes=[B_Va])
                for h in range(4):
                    op("pe", lambda e: e.transpose(out=PT[:, h * 128:(h + 1) * 128], in_=kTp[:, h, tsl], identity=identb[:]),
                       reads=[B_kTp, B_identb], writes=[B_PT])
                op("dve", lambda e: e.tensor_copy(out=kpt[:], in_=PT[:, 0:512].rearrange("p (a b) -> p a b", a=4)),
                   reads=[B_PT], writes=[B_kpt])
                if own:
                    for h in range(4):
                        op("pe", lambda e: e.matmul(PS[2][:, h * 128:(h + 1) * 128], lhsT=kTp[:, h, tsl], rhs=qTp[:, h, tsl],
                                                    start=True, stop=True), reads=[B_kTp, B_qTp], writes=[B_PS[2]])
                    op("dve", lambda e: e.tensor_tensor(out=St[:], in0=PS[2][:].rearrange("p (a b) -> p a b", a=4), in1=maskU_bc, op=ALU.mult),
                       reads=[B_PS[2], B_maskU], writes=[B_St])
                    for h in range(4):
                        bank, B_bank = PS[3 + h // 2], B_PS[3 + h // 2]
                        o = (h % 2) * 129
                        op("pe", lambda e: e.matmul(bank[:, o:o + 129], lhsT=St[:, h, :], rhs=Va[:, h, :], start=True, stop=False),
                           reads=[B_St, B_Va], writes=[B_bank])
                        op("pe", lambda e: e.matmul(bank[:, o:o + 129], lhsT=qTp[:, h, tsl], rhs=Cb[:, h, :], start=False, stop=True),
                           reads=[B_qTp, B_Cb], writes=[B_bank])
                    for bi in range(2):
                        op("act", lambda e: e.activation(out=dn[:, 2 * bi:2 * bi + 2],
                                                         in_=PS[3 + bi][:, 0:258].rearrange("p (a c) -> p a c", c=129)[:, :, 128],
                                                         func=AF.Abs),
                           reads=[B_PS[3 + bi]], writes=[B_dn])
                    op("dve", lambda e: e.tensor_scalar_max(out=dn[:], in0=dn[:], scalar1=1.0), reads=[B_dn], writes=[B_dn])
                    op("dve", lambda e: e.reciprocal(out=dn[:], in_=dn[:]), reads=[B_dn], writes=[B_dn])
                    for h in range(4):
                        bank, B_bank = PS[3 + h // 2], B_PS[3 + h // 2]
                        o = (h % 2) * 129
                        op("act", lambda e: e.activation(out=hm[:, h, :], in_=bank[:, o:o + 128], func=AF.Copy, scale=dn[:, h:h + 1]),
                           reads=[B_bank, B_dn], writes=[B_hm])
                    op("pool", lambda e: e.tensor_tensor(out=sqh[:], in0=hm[:], in1=hm[:], op=ALU.mult), reads=[B_hm], writes=[B_sqh])
                    op("dve", lambda e: e.tensor_reduce(out=ssh[:], in_=sqh[:], axis=AX.X, op=ALU.add), reads=[B_sqh], writes=[B_ssh])
                    op("act", lambda e: e.activation(out=rn[:], in_=ssh[:], func=AF.Sqrt, scale=1.0 / 128, bias=EPS),
                       reads=[B_ssh, B_cst], writes=[B_rn])
                    op("dve", lambda e: e.reciprocal(out=rn[:], in_=rn[:]), reads=[B_rn], writes=[B_rn])
                    op("dve", lambda e: e.tensor_tensor(out=hm[:], in0=hm[:], in1=rn[:].unsqueeze(2).to_broadcast([128, 4, 128]), op=ALU.mult),
                       reads=[B_hm, B_rn], writes=[B_hm])
                    hmf = hm[:].rearrange("p a b -> p (a b)")
                    op("pool", lambda e: e.tensor_tensor(out=hmf, in0=hmf, in1=mnorm[:], op=ALU.mult), reads=[B_hm, B_mnorm], writes=[B_hm])
                    for kc in range(8):
                        op("pe", lambda e: e.matmul(ps[:], lhsT=hTc[:, kc, tsl], rhs=wM[:, kc, M_OM:M_OM + 512],
                                                    start=(kc == 0), stop=(kc == 7)), reads=[B_hTc, B_wM], writes=[B_ps])
                    op("act", lambda e: e.activation(out=so[:], in_=ps[:], func=AF.Sigmoid), reads=[B_ps], writes=[B_so])
                    op("dve", lambda e: e.tensor_tensor(out=ymb[:], in0=hmf, in1=so[:], op=ALU.mult), reads=[B_hm, B_so], writes=[B_ymb])
                    for c in range(4):
                        op("pe", lambda e: e.transpose(out=PT[:, 512 + c * 128:512 + (c + 1) * 128], in_=ymb[:, c * 128:(c + 1) * 128],
                                                       identity=identb[:]), reads=[B_ymb, B_identb], writes=[B_PT])
                    op("dve", lambda e: e.tensor_copy(out=ymT[:, :, tsl], in_=PT[:, 512:1024].rearrange("p (a b) -> p a b", a=4)),
                       reads=[B_PT], writes=[B_ymT])
                for h in range(4):
                    bank, B_bank = PS[5 + h // 2], B_PS[5 + h // 2]
                    o = (h % 2) * 129
                    op("pe", lambda e: e.matmul(bank[:, o:o + 129], lhsT=kpt[:, h, :], rhs=Va[:, h, :], start=True, stop=True),
                       reads=[B_kpt, B_Va], writes=[B_bank])
                for bi in range(2):
                    op("dve", lambda e: e.tensor_tensor(out=Cs[:, 2 * bi:2 * bi + 2, :], in0=PS[5 + bi][:, 0:258].rearrange("p (a c) -> p a c", c=129),
                                                        in1=Cs[:, 2 * bi:2 * bi + 2, :], op=ALU.add),
                       reads=[B_PS[5 + bi], B_Cs], writes=[B_Cs])
                if own:
                    eF_bc, B_eF = bq_g[:, :, ti * 128 + 127: ti * 128 + 128].to_broadcast([128, 4, 129]), B_bq
                else:
                    eF_bc, B_eF = eFs[:, ti, :].unsqueeze(2).to_broadcast([128, 4, 129]), B_eFs
                op("dve", lambda e: e.tensor_tensor(out=Cs[:], in0=Cs[:], in1=eF_bc, op=ALU.mult),
                   reads=[B_Cs, B_eF], writes=[B_Cs])
                op("act", lambda e: e.activation(out=Cb[:], in_=Cs[:], func=AF.Copy), reads=[B_Cs], writes=[B_Cb])
            if own:
                tok0 = (g - 24) * 512
                dma("sp", lambda e: e.dma_start(out=yaT[:], in_=scrYa[:, :, tok0:tok0 + 512]), B_yaT, reads=[B_scrYa], writes=[B_yaT])
                for c in range(8):
                    for kc in range(8):
                        op("pe", lambda e: e.matmul(PS[0][:], lhsT=wM[:, kc, M_GA + c * 128: M_GA + (c + 1) * 128], rhs=hTc[:, kc, :],
                                                    start=(kc == 0), stop=(kc == 7)), reads=[B_hTc, B_wM], writes=[B_PS[0]])
                    op("act", lambda e: e.activation(out=sigA[:], in_=PS[0][:], func=AF.Sigmoid), reads=[B_PS[0]], writes=[B_sigA])
                    for kc in range(8):
                        op("pe", lambda e: e.matmul(PS[1][:], lhsT=wM[:, kc, M_GM + c * 128: M_GM + (c + 1) * 128], rhs=hTc[:, kc, :],
                                                    start=(kc == 0), stop=(kc == 7)), reads=[B_hTc, B_wM], writes=[B_PS[1]])
                    op("act", lambda e: e.activation(out=sigM[:], in_=PS[1][:], func=AF.Sigmoid), reads=[B_PS[1]], writes=[B_sigM])
                    for k4 in range(4):
                        op("pe", lambda e: e.matmul(PS[5][:], lhsT=wUA[:, k4, c * 128:(c + 1) * 128], rhs=yaT[:, k4, :],
                                                    start=(k4 == 0), stop=(k4 == 3)), reads=[B_wUA, B_yaT], writes=[B_PS[5]])
                    for k4 in range(4):
                        op("pe", lambda e: e.matmul(PS[6][:], lhsT=wUM[:, k4, c * 128:(c + 1) * 128], rhs=ymT[:, k4, :],
                                                    start=(k4 == 0), stop=(k4 == 3)), reads=[B_wUM, B_ymT], writes=[B_PS[6]])
                    op("dve", lambda e: e.tensor_tensor(out=sigA[:], in0=sigA[:], in1=PS[5][:], op=ALU.mult),
                       reads=[B_sigA, B_PS[5]], writes=[B_sigA])
                    op("dve", lambda e: e.tensor_tensor(out=sigM[:], in0=sigM[:], in1=PS[6][:], op=ALU.mult),
                       reads=[B_sigM, B_PS[6]], writes=[B_sigM])
                    op("pool", lambda e: e.tensor_tensor(out=mT[:, c, :], in0=sigA[:], in1=sigM[:], op=ALU.add),
                       reads=[B_sigA, B_sigM], writes=[B_mT])
                for ti in range(4):
                    tsl = slice(ti * 128, (ti + 1) * 128)
                    jx = j0 + ti * 128
                    dma("sp", lambda e: e.dma_start(out=x1t[:], in_=xh[jx:jx + 128, :]), B_x1t, writes=[B_x1t])
                    for half in range(2):
                        bank, B_bank = PS[3 + half], B_PS[3 + half]
                        for kc in range(8):
                            op("pe", lambda e: e.matmul(bank[:], lhsT=mT[:, kc, tsl], rhs=wOut[:, kc, half * 512:(half + 1) * 512],
                                                        start=(kc == 0), stop=(kc == 7)), reads=[B_mT, B_wOut], writes=[B_bank])
                        op("dve", lambda e: e.tensor_tensor(out=x1t[:, half * 512:(half + 1) * 512], in0=bank[:],
                                                            in1=x1t[:, half * 512:(half + 1) * 512], op=ALU.add),
                           reads=[B_bank, B_x1t], writes=[B_x1t])
                    dma("sp", lambda e: e.dma_start(out=scrX1[tok0 + ti * 128: tok0 + (ti + 1) * 128, :], in_=x1t[:]),
                        B_x1t, reads=[B_x1t], writes=[])
                    B_scrX1.writes[B_x1t.dsem] = B_x1t.dcount
        f.barrier()
        es.close()
        return B_scrX1

    B_scrX1 = pass_mlstm()
    if stage == 2:
        f.barrier()
        return nc, f


    def pass_peer():
        es = ExitStack()
        wQ = f.sbuf("wQ", [128, 8, 2048], BF16, es); B_wQ = Buf("wQ")
        sg = f.sbuf("sg", [128, 1024], F32, es); B_sg = Buf("sg")
        set_stg([sg[:, 0:512], sg[:, 512:1024]])
        stg, B_stg = stg_cur["t"], stg_cur["B"]
        for i in range(2):
            nrm.append(dict(
                ss=f.sbuf("n_ssp%d" % i, [128, 1], F32, es), B_ss=Buf("n_ss"),
                rs=f.sbuf("n_rsp%d" % i, [128, 1], F32, es), B_rs=Buf("n_rs"),
                xs=f.sbuf("n_xsp%d" % i, [128, 1024], BF16, es), B_xs=Buf("n_xs")))
        load_w(wQ, B_wQ, w_query, 0, 8, 0, 2048, gffn, B_gffn)
        wPG = f.sbuf("wPG", [128, 8, 1024], BF16, es); B_wPG = Buf("wPG")
        load_w(wPG, B_wPG, w_ple_gate, 0, 8, 0, 1024, gple, B_gple)
        wPL = f.sbuf("wPL", [128, 2, 1024], BF16, es); B_wPL = Buf("wPL")
        load_w(wPL, B_wPL, w_ple, 0, 2, 0, 1024)
        gffn_bc = f.sbuf("gffn_bc", [128, 1024], F32, es); B_gffn_bc = Buf("gffn_bc")
        gfin_bc = f.sbuf("gfin_bc", [128, 1024], F32, es); B_gfin_bc = Buf("gfin_bc")
        dma("sp", lambda e: e.dma_start(out=gffn_bc[:], in_=norm_ffn.to_broadcast([128, 1024])), B_gffn_bc, writes=[B_gffn_bc])
        dma("sp", lambda e: e.dma_start(out=gfin_bc[:], in_=norm_final.to_broadcast([128, 1024])), B_gfin_bc, writes=[B_gfin_bc])
        keysT = f.sbuf("keysT", [128, 16, 128], BF16, es); B_keysT = Buf("keysT")
        kst = f.sbuf("kst", [128, 128], BF16, es); B_kst = Buf("kst")
        for j in range(16):
            src = (keys1 if j % 2 == 0 else keys2)[j // 2]
            i = stg_i[0] % 2; stg_i[0] += 1
            dma("sp", lambda e: e.dma_start(out=stg[i][:, 0:128], in_=src), B_stg[i], writes=[B_stg[i]])
            op("act", lambda e: e.activation(out=kst[:], in_=stg[i][:, 0:128], func=AF.Copy), reads=[B_stg[i]], writes=[B_kst])
            op("pe", lambda e: e.transpose(out=PT[:, 0:128], in_=kst[:], identity=identb[:]), reads=[B_kst, B_identb], writes=[B_PT])
            op("dve", lambda e: e.tensor_copy(out=keysT[:, j, :], in_=PT[:, 0:128]), reads=[B_PT], writes=[B_keysT])
        f.barrier()
        iota16 = f.sbuf("iota16", [128, 16], F32, es); B_iota = Buf("iota16")
        thr16 = f.sbuf("thr16", [128, 16], F32, es); B_thr = Buf("thr16")
        for a in range(16):
            op("pool", lambda e: e.memset(iota16[:, a:a + 1], float(a)), writes=[B_iota])
            op("pool", lambda e: e.memset(thr16[:, a:a + 1], float(16 * a)), writes=[B_thr])
        xn = [f.sbuf("xn%d" % i, [128, 1024], BF16, es) for i in range(2)]; B_xn = [Buf("xn0"), Buf("xn1")]
        qT = f.sbuf("qT", [128, 16, 128], BF16, es); B_qT = Buf("qT")
        sccand = f.sbuf("sccand", [128, 2048], F32, es); B_sc = Buf("sccand")
        sc = sccand.rearrange("p (a b) -> p a b", a=16)
        sc2 = f.sbuf("sc2", [128, 128], F32, es); B_sc2 = Buf("sc2")
        tv = f.sbuf("tv", [128, 16, 16], F32, es); B_tv = Buf("tv")
        tix = f.sbuf("tix", [128, 16, 16], U32, es); B_tix = Buf("tix")
        cand = sccand.rearrange("p (a b c) -> p a b c", a=8, b=16); B_cand = B_sc
        cand2 = f.sbuf("cand2", [128, 256], F32, es); B_cand2 = Buf("cand2")
        ts_ = f.sbuf("ts", [128, 8, 16], F32, es); B_ts = Buf("ts")
        tp = f.sbuf("tp", [128, 8, 16], U32, es); B_tp = Buf("tp")
        tpf = f.sbuf("tpf", [128, 8, 16], F32, es); B_tpf = Buf("tpf")
        negmx = f.sbuf("negmx", [128, 8], F32, es); B_negmx = Buf("negmx")
        ge = [f.sbuf("ge%d" % i, [128, 8, 16], F32, es) for i in range(2)]; B_ge = [Buf("ge0"), Buf("ge1")]
        gs = f.sbuf("gs", [128, 8], F32, es); B_gs = Buf("gs")
        i1f = f.sbuf("i1f", [128, 8, 16], F32, es); B_i1f = Buf("i1f")
        i2f = f.sbuf("i2f", [128, 8, 16], F32, es); B_i2f = Buf("i2f")
        d1 = f.sbuf("d1", [128, 8, 16], F32, es); B_d1 = Buf("d1")
        asum = f.sbuf("asum", [128, 8, 16], F32, es); B_asum = Buf("asum")
        bpos = f.sbuf("bpos", [128, 8, 16], F32, es); B_bpos = Buf("bpos")
        i1s = f.sbuf("i1s", [128, 8, 16], F32, es); B_i1s = Buf("i1s")
        i2s = f.sbuf("i2s", [128, 8, 16], F32, es); B_i2s = Buf("i2s")
        eidf = f.sbuf("eidf", [128, 128], F32, es); B_eidf = Buf("eidf")
        eid = [f.sbuf("eid%d" % i, [128, 128], I32, es) for i in range(2)]; B_eid = [Buf("eid0"), Buf("eid1")]
        apre = f.sbuf("apre", [128, 128], F32, es); B_apre = [Buf("apre%d" % i) for i in range(128)]
        gl = f.sbuf("gl", [128, 128], F32, es); B_gl = [Buf("gl%d" % i) for i in range(128)]
        ga = f.sbuf("ga", [128, 128], F32, es); B_ga = [Buf("ga%d" % i) for i in range(128)]
        NB = 14
        gb = [f.sbuf("gb%d" % i, [128, 2048], BF16, es) for i in range(NB)]
        B_gb = [Buf("gb%d" % i) for i in range(NB)]
        junk = f.sbuf("junkp", [128, 1024], BF16, es)
        pt = [f.sbuf("ptile%d" % i, [128, 256], F32, es) for i in range(3)]; B_pt = [Buf("pt0"), Buf("pt1"), Buf("pt2")]
        pb = f.sbuf("pb", [128, 256], BF16, es); B_pb = Buf("pb")
        pT = f.sbuf("pT", [128, 2, 128], BF16, es); B_pT = Buf("pT")
        ot = f.sbuf("ot", [128, 1024], F32, es); B_ot = Buf("ot")
        B_out = Buf("out")
        B_hTs = [Buf("hTs%d" % i) for i in range(4)]
        gcount = [0]

        def top16(src_ap, B_src, tmp, B_tmp, vals, B_vals, idxs, B_idxs):
            op("dve", lambda e: e.max(out=vals[:, 0:8], in_=src_ap), reads=[B_src], writes=[B_vals])
            op("dve", lambda e: e.max_index(out=idxs[:, 0:8], in_max=vals[:, 0:8], in_values=src_ap),
               reads=[B_src, B_vals], writes=[B_idxs])
            op("dve", lambda e: e.match_replace(out=tmp, in_to_replace=vals[:, 0:8], in_values=src_ap, imm_value=-1e30),
               reads=[B_src, B_vals], writes=[B_tmp])
            op("dve", lambda e: e.max(out=vals[:, 8:16], in_=tmp), reads=[B_tmp], writes=[B_vals])
            op("dve", lambda e: e.max_index(out=idxs[:, 8:16], in_max=vals[:, 8:16], in_values=tmp),
               reads=[B_tmp, B_vals], writes=[B_idxs])

        def front(t):
            par = t % 2
            tok0 = t * 128
            x1, B_x1 = xts[t % 4], B_xts[t % 4]
            hs = slice(par * 128, (par + 1) * 128)
            dma("sp", lambda e: e.dma_start(out=x1[:], in_=scrX1[tok0:tok0 + 128, :]), B_x1, reads=[B_scrX1], writes=[B_x1])
            dma("sp", lambda e: e.dma_start(out=pt[t % 3][:], in_=pin[tok0:tok0 + 128, :]), B_pt[t % 3], writes=[B_pt[t % 3]])
            s = yield from norm_T_g(x1[:], B_x1, hT[:, :, hs], B_hTs[par])
            op("dve", lambda e: e.scalar_tensor_tensor(out=xn[par][:], in0=x1[:], scalar=s["rs"][:, 0:1], in1=gffn_bc[:],
                                                       op0=ALU.mult, op1=ALU.mult),
               reads=[B_x1, s["B_rs"], B_gffn_bc], writes=[B_xn[par]])
            yield
            for jb in range(4):
                ps, B_ps = PS[jb % 2], B_PS[jb % 2]
                for jj in range(4):
                    j = 4 * jb + jj
                    for kc in range(8):
                        op("pe", lambda e: e.matmul(ps[:, jj * 128:(jj + 1) * 128], lhsT=wQ[:, kc, j * 128:(j + 1) * 128], rhs=hT[:, kc, hs],
                                                    start=(kc == 0), stop=(kc == 7)), reads=[B_wQ, B_hTs[par]], writes=[B_ps])
                op("act", lambda e: e.activation(out=qT[:, 4 * jb:4 * jb + 4, :], in_=ps[:].rearrange("p (a b) -> p a b", a=4), func=AF.Copy),
                   reads=[B_ps], writes=[B_qT])
                yield
            for jb in range(4):
                ps, B_ps = PS[jb % 2], B_PS[jb % 2]
                for jj in range(4):
                    j = 4 * jb + jj
                    op("pe", lambda e: e.matmul(ps[:, jj * 128:(jj + 1) * 128], lhsT=qT[:, j, :], rhs=keysT[:, j, :], start=True, stop=True),
                       reads=[B_qT, B_keysT], writes=[B_ps])
                op("act", lambda e: e.activation(out=sc[:, 4 * jb:4 * jb + 4, :], in_=ps[:].rearrange("p (a b) -> p a b", a=4), func=AF.Copy),
                   reads=[B_ps], writes=[B_sc])
                yield
            for j in range(16):
                top16(sc[:, j, :], B_sc, sc2[:], B_sc2, tv[:, j, :], B_tv, tix[:, j, :], B_tix)
                yield
            v1 = tv[:, 0:16:2, :].unsqueeze(3).to_broadcast([128, 8, 16, 16])
            v2 = tv[:, 1:16:2, :].unsqueeze(2).to_broadcast([128, 8, 16, 16])
            op("dve", lambda e: e.tensor_tensor(out=cand, in0=v1, in1=v2, op=ALU.add), reads=[B_tv], writes=[B_cand])
            for h in range(8):
                top16(cand[:, h, :, :].rearrange("p a b -> p (a b)"), B_cand, cand2[:], B_cand2, ts_[:, h, :], B_ts, tp[:, h, :], B_tp)
                yield
            op("dve", lambda e: e.tensor_scalar(out=negmx[:], in0=ts_[:, :, 0], scalar1=-1.0, scalar2=None, op0=ALU.mult),
               reads=[B_ts], writes=[B_negmx])
            op("pool", lambda e: e.memset(gs[:], 0.0), writes=[B_gs])
            gep, B_gep = ge[par], B_ge[par]
            for h in range(8):
                op("act", lambda e: e.activation(out=gep[:, h, :], in_=ts_[:, h, :], func=AF.Exp, bias=negmx[:, h:h + 1],
                                                 accum_out=gs[:, h:h + 1]),
                   reads=[B_ts, B_negmx, B_gs], writes=[B_gep, B_gs])
            yield
            op("dve", lambda e: e.reciprocal(out=gs[:], in_=gs[:]), reads=[B_gs], writes=[B_gs])
            op("dve", lambda e: e.tensor_tensor(out=gep[:], in0=gep[:], in1=gs[:].unsqueeze(2).to_broadcast([128, 8, 16]), op=ALU.mult),
               reads=[B_gep, B_gs], writes=[B_gep])
            yield
            op("dve", lambda e: e.tensor_copy(out=tpf[:], in_=tp[:]), reads=[B_tp], writes=[B_tpf])
            op("dve", lambda e: e.tensor_copy(out=i1f[:], in_=tix[:, 0:16:2, :]), reads=[B_tix], writes=[B_i1f])
            op("dve", lambda e: e.tensor_copy(out=i2f[:], in_=tix[:, 1:16:2, :]), reads=[B_tix], writes=[B_i2f])
            op("dve", lambda e: e.tensor_copy(out=d1[:, :, 0:1], in_=i1f[:, :, 0:1]), reads=[B_i1f], writes=[B_d1])
            op("dve", lambda e: e.tensor_tensor(out=d1[:, :, 1:16], in0=i1f[:, :, 1:16], in1=i1f[:, :, 0:15], op=ALU.subtract),
               reads=[B_i1f], writes=[B_d1])
            pos_bc = tpf[:].unsqueeze(3).to_broadcast([128, 8, 16, 16])
            thr_bc = thr16[:].unsqueeze(1).unsqueeze(1).to_broadcast([128, 8, 16, 16])
            iota_bc = iota16[:].unsqueeze(1).unsqueeze(1).to_broadcast([128, 8, 16, 16])
            op("dve", lambda e: e.tensor_tensor(out=cand, in0=pos_bc, in1=thr_bc, op=ALU.is_ge), reads=[B_tpf, B_thr], writes=[B_cand])
            op("dve", lambda e: e.tensor_reduce(out=asum[:], in_=cand, axis=AX.X, op=ALU.add), reads=[B_cand], writes=[B_asum])
            op("dve", lambda e: e.tensor_tensor(out=cand, in0=cand, in1=d1[:].unsqueeze(2).to_broadcast([128, 8, 16, 16]), op=ALU.mult),
               reads=[B_cand, B_d1], writes=[B_cand])
            op("dve", lambda e: e.tensor_reduce(out=i1s[:], in_=cand, axis=AX.X, op=ALU.add), reads=[B_cand], writes=[B_i1s])
            yield
            op("dve", lambda e: e.scalar_tensor_tensor(out=bpos[:], in0=asum[:], scalar=-16.0, in1=tpf[:], op0=ALU.mult, op1=ALU.add),
               reads=[B_asum, B_tpf], writes=[B_bpos])
            op("dve", lambda e: e.tensor_scalar_add(out=bpos[:], in0=bpos[:], scalar1=16.0), reads=[B_bpos], writes=[B_bpos])
            op("dve", lambda e: e.tensor_tensor(out=cand, in0=bpos[:].unsqueeze(3).to_broadcast([128, 8, 16, 16]), in1=iota_bc, op=ALU.is_equal),
               reads=[B_bpos, B_iota], writes=[B_cand])
            op("dve", lambda e: e.tensor_tensor(out=cand, in0=cand, in1=i2f[:].unsqueeze(2).to_broadcast([128, 8, 16, 16]), op=ALU.mult),
               reads=[B_cand, B_i2f], writes=[B_cand])
            op("dve", lambda e: e.tensor_reduce(out=i2s[:], in_=cand, axis=AX.X, op=ALU.add), reads=[B_cand], writes=[B_i2s])
            op("dve", lambda e: e.scalar_tensor_tensor(out=eidf[:], in0=i1s[:].rearrange("p a b -> p (a b)"), scalar=128.0,
                                                       in1=i2s[:].rearrange("p a b -> p (a b)"), op0=ALU.mult, op1=ALU.add),
               reads=[B_i1s, B_i2s], writes=[B_eidf])
            op("dve", lambda e: e.tensor_copy(out=eid[par][:], in_=eidf[:]), reads=[B_eidf], writes=[B_eid[par]])

        dg = [f.sbuf("dg%d" % i, [128, 128], BF16, es) for i in range(4)]
        B_dg = [Buf("dg%d" % i) for i in range(4)]

        def slots(t, bgen=None):
            par = t % 2
            op("pool", lambda e: e.memset(apre[:], 0.0), writes=B_apre)
            gef = ge[par][:].rearrange("p a b -> p (a b)")
            gen = None
            for sl in range(128):
                i = gcount[0] % NB; gcount[0] += 1
                k = sl % 4
                dma("pool", lambda e: e.indirect_dma_start(out=gb[i][:], out_offset=None, in_=scrUV,
                                                           in_offset=bass.IndirectOffsetOnAxis(ap=eid[par][:, sl:sl + 1], axis=0)),
                    B_gb[i], reads=[B_eid[par], B_uv], writes=[B_gb[i]])
                op("dve", lambda e: e.scalar_tensor_tensor(out=junk[:], in0=gb[i][:, 0:1024], scalar=1.0, in1=xn[par][:],
                                                           op0=ALU.mult, op1=ALU.mult, accum_out=apre[:, sl:sl + 1]),
                   reads=[B_gb[i], B_xn[par], B_apre[sl]], writes=[B_apre[sl]], skip_self=True)
                op("act", lambda e: e.activation(out=gl[:, sl:sl + 1], in_=apre[:, sl:sl + 1], func=AF.Gelu),
                   reads=[B_apre[sl]], writes=[B_gl[sl]])
                op("act", lambda e: e.activation(out=ga[:, sl:sl + 1], in_=gl[:, sl:sl + 1], func=AF.Copy, scale=gef[:, sl:sl + 1]),
                   reads=[B_gl[sl], B_ge[par]], writes=[B_ga[sl]])
                op("act", lambda e: e.activation(out=dg[k][:], in_=identf[:], func=AF.Copy, scale=ga[:, sl:sl + 1]),
                   reads=[B_identf, B_ga[sl]], writes=[B_dg[k]])
                for half in range(2):
                    op("pe", lambda e: e.matmul(PS[5 + half][:], lhsT=dg[k][:], rhs=gb[i][:, 1024 + half * 512:1024 + (half + 1) * 512],
                                                start=(sl == 0), stop=(sl == 127)), reads=[B_dg[k], B_gb[i]], writes=[B_PS[5 + half]])
                if sl == 3 and t + 1 < NTOK // 128:
                    gen = front(t + 1)
                if bgen is not None and sl % 2 == 1:
                    try:
                        next(bgen)
                    except StopIteration:
                        bgen = None
                if gen is not None and sl % 3 == 0:
                    try:
                        next(gen)
                    except StopIteration:
                        gen = None
            assert gen is None and bgen is None

        PT2 = PS[4][:].bitcast(BF16)
        B_PT2 = B_PS[4]

        def back(t):
            par = t % 2
            tok0 = t * 128
            x1, B_x1 = xts[t % 4], B_xts[t % 4]
            hs = slice(256 + par * 128, 256 + (par + 1) * 128)
            B_h = B_hTs[2 + par]
            for half in range(2):
                op("dve", lambda e: e.tensor_tensor(out=x1[:, half * 512:(half + 1) * 512], in0=PS[5 + half][:],
                                                    in1=x1[:, half * 512:(half + 1) * 512], op=ALU.add),
                   reads=[B_PS[5 + half], B_x1], writes=[B_x1])
            yield
            yield from norm_T_g(x1[:], B_x1, hT[:, :, hs], B_h, PT2, B_PT2)
            yield
            for half in range(2):
                for kc in range(8):
                    op("pe", lambda e: e.matmul(PS[2 + half][:], lhsT=hT[:, kc, hs], rhs=wPG[:, kc, half * 512:(half + 1) * 512],
                                                start=(kc == 0), stop=(kc == 7)), reads=[B_h, B_wPG], writes=[B_PS[2 + half]])
                op("act", lambda e: e.activation(out=sg[:, half * 512:(half + 1) * 512], in_=PS[2 + half][:], func=AF.Sigmoid),
                   reads=[B_PS[2 + half]], writes=[B_sg])
            op("act", lambda e: e.activation(out=pb[:], in_=pt[t % 3][:], func=AF.Copy), reads=[B_pt[t % 3]], writes=[B_pb])
            for c in range(2):
                op("pe", lambda e: e.transpose(out=PT2[:, c * 128:(c + 1) * 128], in_=pb[:, c * 128:(c + 1) * 128], identity=identb[:]),
                   reads=[B_pb, B_identb], writes=[B_PT2])
            yield
            op("dve", lambda e: e.tensor_copy(out=pT[:], in_=PT2[:, 0:256].rearrange("p (a b) -> p a b", a=2)), reads=[B_PT2], writes=[B_pT])
            for half in range(2):
                for c in range(2):
                    op("pe", lambda e: e.matmul(PS[2 + half][:], lhsT=pT[:, c, :], rhs=wPL[:, c, half * 512:(half + 1) * 512],
                                                start=(c == 0), stop=(c == 1)), reads=[B_pT, B_wPL], writes=[B_PS[2 + half]])
            yield
            for half in range(2):
                hsl = slice(half * 512, (half + 1) * 512)
                op("dve", lambda e: e.tensor_tensor(out=sg[:, hsl], in0=sg[:, hsl], in1=PS[2 + half][:], op=ALU.mult),
                   reads=[B_sg, B_PS[2 + half]], writes=[B_sg])
                op("dve", lambda e: e.tensor_tensor(out=x1[:, hsl], in0=x1[:, hsl], in1=sg[:, hsl], op=ALU.add),
                   reads=[B_x1, B_sg], writes=[B_x1])
            s4 = yield from rms_g(x1[:], B_x1)
            op("dve", lambda e: e.scalar_tensor_tensor(out=ot[:], in0=x1[:], scalar=s4["rs"][:, 0:1], in1=gfin_bc[:],
                                                       op0=ALU.mult, op1=ALU.mult),
               reads=[B_x1, s4["B_rs"], B_gfin_bc], writes=[B_ot])
            dma("sp", lambda e: e.dma_start(out=out_d[tok0:tok0 + 128, :], in_=ot[:]), B_ot, reads=[B_ot], writes=[])
            B_out.writes[B_ot.dsem] = B_ot.dcount

        for _ in front(0):
            pass
        bg = None
        for t in range(NTOK // 128):
            slots(t, bg)
            bg = back(t)
            next(bg)
        for _ in bg:
            pass
        f.barrier()
        es.close()

    pass_peer()
    f.barrier()
    return nc, f


_W_KEYS = ["w_in", "norm_mix", "conv_qk", "gate_bias", "mlstm_norm", "w_up_attn", "w_up_mlstm", "w_out",
           "norm_ffn", "w_query", "keys1", "keys2", "expert_u", "expert_v", "norm_ple", "w_ple_gate", "w_ple"]


def make_in_maps(inputs, cores=range(8)):
    x = np.asarray(inputs["x"], dtype=np.float32)
    p = np.asarray(inputs["p"], dtype=np.float32)[0]
    shared = {}
    for k in _W_KEYS:
        a = np.asarray(inputs[k], dtype=np.float32)[0]
        if k in ("norm_mix", "norm_ple"):
            shared[k] = np.ascontiguousarray(a)
        elif a.ndim == 1:
            shared[k] = np.ascontiguousarray(a[None, :])
        else:
            shared[k] = np.ascontiguousarray(a)
    shared["norm_final"] = np.ascontiguousarray(np.asarray(inputs["norm_final"], dtype=np.float32)[None, :])
    maps = []
    for c in cores:
        b, seg = c // 4, c % 4
        s0 = seg * NTOK
        xhh = np.zeros((NHIST + NTOK, 1024), np.float32)
        if s0 > 0:
            xhh[NHIST - s0:NHIST] = x[b, 0:s0]
        xhh[NHIST:] = x[b, s0:s0 + NTOK]
        m = dict(shared)
        m["xh"] = xhh
        m["p"] = np.ascontiguousarray(p[b, s0:s0 + NTOK])
        m["hv"] = np.full((128, 1), 1.0 if seg > 0 else 0.0, np.float32)
        maps.append(m)
    return maps


def kernel(**inputs):
    nc, f = build()
    maps = make_in_maps(inputs)
    res = run_bass_kernel_spmd(nc, maps, core_ids=list(range(8)))
    out = np.zeros((2, 4 * NTOK, 1024), np.float32)
    for c in range(8):
        b, seg = c // 4, c % 4
        out[b, seg * NTOK:(seg + 1) * NTOK] = res.results[c]["out"]
    return out
```

```python
from contextlib import ExitStack
import numpy as np
import concourse.bass as bass
import concourse.mybir as mybir
from concourse.bass_utils import run_bass_kernel_spmd

F32 = mybir.dt.float32
BF16 = mybir.dt.bfloat16
U32 = mybir.dt.uint32
I32 = mybir.dt.int32
ALU = mybir.AluOpType
AF = mybir.ActivationFunctionType
AX = mybir.AxisListType

SEM_MAX = 30000
NTOK = 4096
NHIST = 12288
HALO = 2048


class Buf:
    __slots__ = ("name", "writes", "reads", "dsem", "dcount")

    def __init__(self, name):
        self.name = name
        self.writes = {}
        self.reads = {}
        self.dsem = None
        self.dcount = 0


class FW:
    def __init__(self, nc, same_engine_sync=True):
        self.nc = nc
        self.es = ExitStack()
        self.same = same_engine_sync
        self.eng = {"pe": nc.tensor, "act": nc.scalar, "dve": nc.vector,
                    "pool": nc.gpsimd, "sp": nc.sync}
        self.sems = {}
        self.cnt = {}
        self.ekey = {}
        self.nep = 0
        for k in self.eng:
            self.ekey[k] = k + "#0"
            self.sems[self.ekey[k]] = self.es.enter_context(nc.semaphore("s_" + k + "_0"))
            self.cnt[k] = 0
        self.known = {k: {} for k in self.eng}
        self.nd = 0
        self.ninstr = 0
        self.dmax = {}

    def sbuf(self, name, shape, dt, es=None):
        return (es or self.es).enter_context(self.nc.sbuf_tensor(name, list(shape), dt))

    def psum(self, name, shape, dt):
        return self.es.enter_context(self.nc.psum_tensor(name, list(shape), dt))

    def buf(self, name):
        return Buf(name)

    def _dsem(self, b):
        if b.dsem is None or b.dcount + 16 > SEM_MAX:
            b.dcount = 0
            key = "d%d" % self.nd
            self.nd += 1
            self.sems[key] = self.es.enter_context(self.nc.semaphore(key))
            b.dsem = key
        return b.dsem

    def _waits(self, e, reads, writes, extra=None, skip_self=False):
        need = dict(extra) if extra else {}
        for b in reads:
            for s, v in b.writes.items():
                if need.get(s, 0) < v:
                    need[s] = v
        for b in writes:
            for s, v in b.writes.items():
                if need.get(s, 0) < v:
                    need[s] = v
            for s, v in b.reads.items():
                if need.get(s, 0) < v:
                    need[s] = v
        kn = self.known[e]
        eng = self.eng[e]
        for s, v in need.items():
            if s.split("#")[0] == e and (e == "pe" or not self.same or skip_self):
                continue
            if kn.get(s, 0) >= v:
                continue
            eng.wait_ge(self.sems[s], v)
            kn[s] = v
            self.ninstr += 1

    def op(self, e, ins, reads=(), writes=(), skip_self=False):
        self._waits(e, reads, writes, skip_self=skip_self)
        i = ins(self.eng[e])
        if self.cnt[e] >= SEM_MAX:
            self.nep += 1
            self.ekey[e] = "%s#%d" % (e, self.nep)
            self.sems[self.ekey[e]] = self.es.enter_context(
                self.nc.semaphore("s_%s_%d" % (e, self.nep)))
            self.cnt[e] = 0
        ek = self.ekey[e]
        self.cnt[e] += 1
        c = self.cnt[e]
        i.then_inc(self.sems[ek], 1)
        self.ninstr += 1
        for b in writes:
            b.writes = {ek: c}
            b.reads = {}
        for b in reads:
            if b.reads.get(ek, 0) < c:
                b.reads[ek] = c
        return i

    def dma(self, q, ins, side, reads=(), writes=()):
        self._waits(q, reads, writes)
        i = ins(self.eng[q])
        s = self._dsem(side)
        side.dcount += 16
        c = side.dcount
        i.then_inc(self.sems[s], 16)
        self.dmax[s] = c
        self.ninstr += 1
        for b in writes:
            b.writes = {s: c}
            b.reads = {}
        for b in reads:
            if b.reads.get(s, 0) < c:
                b.reads[s] = c
        return i

    def barrier(self):
        tot = {self.ekey[k]: self.cnt[k] for k in self.eng if self.cnt[k] > 0}
        tot.update(self.dmax)
        for e in self.eng:
            self._waits(e, (), (), extra=tot)

    def wait_all(self, e, bufs):
        self._waits(e, bufs, bufs)


C_QA, C_KA, C_VA = 0, 512, 1024
C_QKM, C_VM, C_OM, C_IF, C_GA, C_GM = 1536, 2560, 3072, 3584, 3592, 4616
N_IN = 5640


def build(stage=99):
    nc = bass.Bass("TRN2", target_bir_lowering=False)

    def din(name, shape, dt=F32):
        return nc.dram_tensor(name, list(shape), dt, kind="ExternalInput").ap()

    xh = din("xh", [NHIST + NTOK, 1024])
    pin = din("p", [NTOK, 256])
    hv_d = din("hv", [128, 1])
    w_in = din("w_in", [1024, N_IN])
    norm_mix = din("norm_mix", [1024])
    conv_qk = din("conv_qk", [4, 1024])
    gate_bias = din("gate_bias", [1, 8])
    mlstm_norm = din("mlstm_norm", [1, 512])
    w_up_attn = din("w_up_attn", [512, 1024])
    w_up_mlstm = din("w_up_mlstm", [512, 1024])
    w_out = din("w_out", [1024, 1024])
    norm_ffn = din("norm_ffn", [1, 1024])
    w_query = din("w_query", [1024, 2048])
    keys1 = din("keys1", [8, 128, 128])
    keys2 = din("keys2", [8, 128, 128])
    expert_u = din("expert_u", [16384, 1024])
    expert_v = din("expert_v", [16384, 1024])
    norm_ple = din("norm_ple", [1024])
    w_ple_gate = din("w_ple_gate", [1024, 1024])
    w_ple = din("w_ple", [256, 1024])
    norm_final = din("norm_final", [1, 1024])
    out_d = nc.dram_tensor("out", [NTOK, 1024], F32, kind="ExternalOutput").ap()
    scrV = nc.dram_tensor("scrV", [HALO + NTOK, 8, 128], BF16, kind="Internal").ap()
    scrYa = nc.dram_tensor("scrYa", [128, 4, NTOK], BF16, kind=("ExternalOutput" if stage == 1 else "Internal")).ap()
    scrUV = nc.dram_tensor("scrUV", [16384, 2048], BF16, kind="Internal").ap()
    scrX1 = nc.dram_tensor("scrX1", [NTOK, 1024], F32, kind=("ExternalOutput" if stage == 2 else "Internal")).ap()

    f = FW(nc)
    op, dma = f.op, f.dma

    cst = f.sbuf("cst", [128, 8], F32); B_cst = Buf("cst")
    identf = f.sbuf("identf", [128, 128], F32); B_identf = Buf("identf")
    identb = f.sbuf("identb", [128, 128], BF16); B_identb = Buf("identb")
    maskU = f.sbuf("maskU", [128, 128], F32); B_maskU = Buf("maskU")
    maskL = f.sbuf("maskL", [128, 128], F32); B_maskL = Buf("maskL")
    onesf = f.sbuf("onesf", [128, 128], F32); B_onesf = Buf("onesf")
    hv = f.sbuf("hvt", [128, 1], F32); B_hv = Buf("hv")
    EPS, ONE = cst[:, 0:1], cst[:, 1:2]

    op("pool", lambda e: e.memset(cst[:, 0:1], 1e-6), writes=[B_cst])
    op("pool", lambda e: e.memset(cst[:, 1:2], 1.0), writes=[B_cst])
    op("pool", lambda e: e.memset(cst[:, 2:8], 0.0), writes=[B_cst])
    op("pool", lambda e: e.memset(onesf[:], 1.0), writes=[B_onesf])
    op("pool", lambda e: e.memset(identf[:], 1.0), writes=[B_identf])
    op("pool", lambda e: e.affine_select(out=identf[:], in_=identf[:], pattern=[[-1, 128]],
                                         compare_op=ALU.is_equal, fill=0.0, base=0, channel_multiplier=1),
       reads=[B_identf], writes=[B_identf])
    op("dve", lambda e: e.tensor_copy(out=identb[:], in_=identf[:]), reads=[B_identf], writes=[B_identb])
    op("pool", lambda e: e.memset(maskU[:], 1.0), writes=[B_maskU])
    op("pool", lambda e: e.affine_select(out=maskU[:], in_=maskU[:], pattern=[[1, 128]],
                                         compare_op=ALU.is_ge, fill=0.0, base=0, channel_multiplier=-1),
       reads=[B_maskU], writes=[B_maskU])
    op("pool", lambda e: e.memset(maskL[:], 1.0), writes=[B_maskL])
    op("pool", lambda e: e.affine_select(out=maskL[:], in_=maskL[:], pattern=[[-1, 128]],
                                         compare_op=ALU.is_ge, fill=0.0, base=0, channel_multiplier=1),
       reads=[B_maskL], writes=[B_maskL])
    dma("sp", lambda e: e.dma_start(out=hv[:], in_=hv_d), B_hv, writes=[B_hv])

    PS = [f.psum("ps%d" % i, [128, 512], F32) for i in range(7)]
    B_PS = [Buf("ps%d" % i) for i in range(7)]
    PT = f.psum("pt", [128, 1024], BF16); B_PT = Buf("pt")

    NSET = 2
    n_jk = f.sbuf("n_jk", [128, 1024], BF16)
    nrm = []
    for i in range(NSET):
        nrm.append(dict(
            ss=f.sbuf("n_ss%d" % i, [128, 1], F32), B_ss=Buf("n_ss"),
            rs=f.sbuf("n_rs%d" % i, [128, 1], F32), B_rs=Buf("n_rs"),
            xs=f.sbuf("n_xs%d" % i, [128, 1024], BF16), B_xs=Buf("n_xs")))
    nrm_i = [0]

    def _run(g):
        try:
            while True:
                next(g)
        except StopIteration as e:
            return e.value

    def rms_g(xt_ap, B_x, width=1024):
        s = nrm[nrm_i[0] % len(nrm)]; nrm_i[0] += 1
        op("act", lambda e: e.activation(out=n_jk[:, 0:width], in_=xt_ap, func=AF.Square, accum_out=s["ss"][:]),
           reads=[B_x], writes=[s["B_ss"]])
        op("act", lambda e: e.activation(out=s["rs"][:], in_=s["ss"][:], func=AF.Sqrt, scale=1.0 / width, bias=EPS),
           reads=[s["B_ss"], B_cst], writes=[s["B_rs"]])
        yield
        op("dve", lambda e: e.reciprocal(out=s["rs"][:], in_=s["rs"][:]), reads=[s["B_rs"]], writes=[s["B_rs"]])
        return s

    def norm_T_g(xt_ap, B_x, dst_ap, B_dst, pt_ap=None, B_pt_=None):
        if pt_ap is None:
            pt_ap, B_pt_ = PT[:], B_PT
        s = yield from rms_g(xt_ap, B_x)
        op("act", lambda e: e.activation(out=s["xs"][:], in_=xt_ap, func=AF.Copy, scale=s["rs"][:, 0:1]),
           reads=[B_x, s["B_rs"]], writes=[s["B_xs"]])
        for c in range(8):
            op("pe", lambda e: e.transpose(out=pt_ap[:, c * 128:(c + 1) * 128], in_=s["xs"][:, c * 128:(c + 1) * 128],
                                           identity=identb[:]), reads=[s["B_xs"], B_identb], writes=[B_pt_])
        yield
        op("dve", lambda e: e.tensor_copy(out=dst_ap, in_=pt_ap.rearrange("p (c t) -> p c t", c=8)),
           reads=[B_pt_], writes=[B_dst])
        return s

    def rms(xt_ap, B_x, width=1024):
        return _run(rms_g(xt_ap, B_x, width))

    def norm_T(xt_ap, B_x, dst_ap, B_dst):
        return _run(norm_T_g(xt_ap, B_x, dst_ap, B_dst))

    stg_i = [0]
    stg_cur = dict(t=None, B=None)

    def set_stg(aps):
        stg_cur["t"] = aps
        stg_cur["B"] = [Buf("stg%d" % i) for i in range(len(aps))]

    def load_w(dst, B_dst, src, r0, nchunk, c0, ncol, gain=None, B_gain=None, dcol0=0):
        stg, B_stg = stg_cur["t"], stg_cur["B"]
        for kc in range(nchunk):
            for cc in range(0, ncol, 512):
                w = min(512, ncol - cc)
                i = stg_i[0] % len(stg); stg_i[0] += 1
                dma("sp", lambda e: e.dma_start(out=stg[i][:, 0:w], in_=src[r0 + kc * 128: r0 + (kc + 1) * 128, c0 + cc: c0 + cc + w]),
                    B_stg[i], writes=[B_stg[i]])
                if gain is None:
                    op("act", lambda e: e.activation(out=dst[:, kc, dcol0 + cc: dcol0 + cc + w], in_=stg[i][:, 0:w], func=AF.Copy),
                       reads=[B_stg[i]], writes=[B_dst])
                else:
                    op("act", lambda e: e.activation(out=dst[:, kc, dcol0 + cc: dcol0 + cc + w], in_=stg[i][:, 0:w], func=AF.Copy,
                                                     scale=gain[:, kc:kc + 1]),
                       reads=[B_stg[i], B_gain], writes=[B_dst])

    gmix = f.sbuf("gmix", [128, 8], F32); B_gmix = Buf("gmix")
    gple = f.sbuf("gple", [128, 8], F32); B_gple = Buf("gple")
    gffn = f.sbuf("gffn", [128, 8], F32); B_gffn = Buf("gffn")
    dma("sp", lambda e: e.dma_start(out=gmix[:], in_=norm_mix.rearrange("(c p) -> p c", p=128), allow_slow_non_contiguous=True),
        B_gmix, writes=[B_gmix])
    dma("sp", lambda e: e.dma_start(out=gple[:], in_=norm_ple.rearrange("(c p) -> p c", p=128), allow_slow_non_contiguous=True),
        B_gple, writes=[B_gple])
    dma("sp", lambda e: e.dma_start(out=gffn[:], in_=norm_ffn[0].rearrange("(c p) -> p c", p=128), allow_slow_non_contiguous=True),
        B_gffn, writes=[B_gffn])

    xts = [f.sbuf("xt%d" % i, [128, 1024], F32) for i in range(4)]
    B_xts = [Buf("xt%d" % i) for i in range(4)]
    hT = f.sbuf("hT", [128, 8, 512], BF16); B_hT = Buf("hT")

    def pass_attention():
        es = ExitStack()
        wA = f.sbuf("wA", [128, 8, 1536], BF16, es); B_wA = Buf("wA")
        accN = f.sbuf("accN", [128, 2, 2048], F32, es); B_accN = Buf("accN")
        set_stg([accN[:, a, b * 512:(b + 1) * 512] for a in range(2) for b in range(4)])
        load_w(wA, B_wA, w_in, 0, 8, 0, 1536, gmix, B_gmix)
        f.barrier()
        hT2 = f.sbuf("hT2", [128, 8, 512], BF16, es)
        hTb = [hT, hT2]; B_hTb = [Buf("hTa"), Buf("hTb")]
        kT = f.sbuf("kT", [128, 4, HALO + NTOK], BF16, es)
        B_kT = [Buf("kT%d" % i) for i in range(3)]
        qp = f.sbuf("qp", [128, 8, 2048], BF16, es); B_qp = Buf("qp")
        op("pool", lambda e: e.memset(qp[:], 0.0), writes=[B_qp])
        vpad = [f.sbuf("vpad%d" % i, [128, 8, 128], BF16, es) for i in range(2)]
        B_vpad = [Buf("vpad0"), Buf("vpad1")]
        for i in range(2):
            op("pool", lambda e: e.memset(vpad[i][:], 0.0), writes=[B_vpad[i]])
        B_scrV = [Buf("scrV%d" % i) for i in range(3)]
        onespad = f.sbuf("onespad", [128, 2, 128], BF16, es); B_op = Buf("onespad")
        op("pool", lambda e: e.memset(onespad[:], 0.0), writes=[B_op])
        op("pool", lambda e: e.memset(onespad[:, 0, 0:64], 1.0), writes=[B_op])
        op("pool", lambda e: e.memset(onespad[:, 1, 64:128], 1.0), writes=[B_op])
        amN = f.sbuf("amN", [128, 2, 2, 128], F32, es); B_amN = Buf("amN")
        amH = f.sbuf("amH", [128, 2, 2, 128], F32, es); B_amH = Buf("amH")
        for hh in range(2):
            op("dve", lambda e: e.tensor_copy(out=amN[:, hh, 0, :], in_=maskL[:]), reads=[B_maskL], writes=[B_amN])
            op("dve", lambda e: e.tensor_copy(out=amN[:, hh, 1, :], in_=maskU[:]), reads=[B_maskU], writes=[B_amN])
            op("dve", lambda e: e.tensor_scalar(out=amH[:, hh, 0, :], in0=maskL[:], scalar1=hv[:, 0:1], scalar2=None, op0=ALU.mult),
               reads=[B_maskL, B_hv], writes=[B_amH])
            op("dve", lambda e: e.tensor_copy(out=amH[:, hh, 1, :], in_=maskU[:]), reads=[B_maskU], writes=[B_amH])
        accD = f.sbuf("accD", [128, 2, 2048], F32, es); B_accD = Buf("accD")
        yb = f.sbuf("yb", [128, 2, 2048], BF16, es); B_yb = Buf("yb")
        Et = [f.sbuf("Et%d" % i, [128, 512], F32, es) for i in range(2)]
        B_Et = [Buf("Et0"), Buf("Et1")]
        Pb = [f.sbuf("Pb%d" % i, [128, 512], BF16, es) for i in range(2)]
        B_Pb = [Buf("Pb0"), Buf("Pb1")]
        vt = [f.sbuf("vt%d" % i, [128, 4, 128], BF16, es) for i in range(3)]
        B_vt = [Buf("vt%d" % i) for i in range(3)]
        B_scrYa = Buf("scrYa")
        cnt = dict(e=0, v=0)

        def attention(QB):
            uW = QB * 2048
            for half in range(2):
                op("pool", lambda e: e.memset(accN[:], 0.0), writes=[B_accN])
                op("pool", lambda e: e.memset(accD[:], 0.0), writes=[B_accD])
                units = []
                for d in (1, 4, 16):
                    span = 128 * d
                    m0 = (uW + 2048) // span
                    nm = 2048 // span
                    for r in range(d):
                        for m in range(m0, m0 + nm):
                            for hp2 in range(2):
                                units.append((d, r, m, hp2, m == m0))
                st = dict(iprev=None, icur=None)
                nb, db = PS[4], PS[5]

                def load_v(d, r, m):
                    span = 128 * d
                    i = cnt["v"] % 3; cnt["v"] += 1
                    u0 = span * m + r
                    blk = u0 // 2048
                    dma("sp", lambda e: e.dma_start(out=vt[i][:], in_=scrV[u0: u0 + span - d + 1: d, 4 * half: 4 * half + 4, :]),
                        B_vt[i], reads=[B_scrV[blk]], writes=[B_vt[i]])
                    return i

                def emitS(idx):
                    d, r, m, hp2, first = units[idx]
                    span = 128 * d
                    hp = 2 * half + hp2
                    sb = idx % 2
                    sps, B_sps = PS[2 + sb], B_PS[2 + sb]
                    uq = span * m + r - (uW + 2048)
                    qsl = slice(uq, uq + span - d + 1, d)
                    for hh in range(2):
                        h = 2 * hp + hh
                        for blk in range(2):
                            uk = span * (m - 1 + blk) + r
                            kb = uk // 2048
                            o = (hh * 2 + blk) * 128
                            op("pe", lambda e: e.matmul(sps[:, o:o + 128], lhsT=kT[:, hp, uk: uk + span - d + 1: d],
                                                        rhs=qp[:, h, qsl], start=True, stop=True),
                               reads=[B_kT[kb], B_qp], writes=[B_sps])

                def emitEM(idx):
                    d, r, m, hp2, first = units[idx]
                    span = 128 * d
                    sb = idx % 2
                    sps, B_sps = PS[2 + sb], B_PS[2 + sb]
                    prev_halo = (span * (m - 1)) < 2048
                    op("act", lambda e: e.activation(out=Et[sb][:], in_=sps[:], func=AF.Exp, scale=0.125),
                       reads=[B_sps], writes=[B_Et[sb]])
                    am, B_am = (amH, B_amH) if prev_halo else (amN, B_amN)
                    op("dve", lambda e: e.tensor_tensor(out=Pb[sb][:], in0=Et[sb][:],
                                                        in1=am[:].rearrange("p a b q -> p (a b q)"), op=ALU.mult),
                       reads=[B_Et[sb], B_am], writes=[B_Pb[sb]])

                def emitPV(idx):
                    d, r, m, hp2, first = units[idx]
                    span = 128 * d
                    sb = idx % 2
                    if hp2 == 0:
                        if first:
                            st["iprev"] = load_v(d, r, m - 1)
                        st["icur"] = load_v(d, r, m)
                    vsl = (st["iprev"], st["icur"])
                    k = 0
                    for hh in range(2):
                        for blk in range(2):
                            o = (hh * 2 + blk) * 128
                            op("pe", lambda e: e.matmul(nb[:, hp2 * 128:(hp2 + 1) * 128],
                                                        lhsT=vt[vsl[blk]][:, 2 * hp2 + hh, :],
                                                        rhs=Pb[sb][:, o:o + 128], start=(k == 0), stop=(k == 3)),
                               reads=[B_vt[vsl[blk]], B_Pb[sb]], writes=[B_PS[4]])
                            k += 1
                    k = 0
                    for hh in range(2):
                        for blk in range(2):
                            o = (hh * 2 + blk) * 128
                            op("pe", lambda e: e.matmul(db[:, hp2 * 128:(hp2 + 1) * 128],
                                                        lhsT=onespad[:, hh, :],
                                                        rhs=Pb[sb][:, o:o + 128], start=(k == 0), stop=(k == 3)),
                               reads=[B_op, B_Pb[sb]], writes=[B_PS[5]])
                            k += 1
                    if hp2 == 1:
                        uq = span * m + r - (uW + 2048)
                        qsl = slice(uq, uq + span - d + 1, d)
                        op("dve", lambda e: e.tensor_tensor(out=accN[:, :, qsl], in0=nb[:, 0:256].rearrange("p (a q) -> p a q", a=2),
                                                            in1=accN[:, :, qsl], op=ALU.add),
                           reads=[B_PS[4], B_accN], writes=[B_accN])
                        op("dve", lambda e: e.tensor_tensor(out=accD[:, :, qsl], in0=db[:, 0:256].rearrange("p (a q) -> p a q", a=2),
                                                            in1=accD[:, :, qsl], op=ALU.add),
                           reads=[B_PS[5], B_accD], writes=[B_accD])
                        st["iprev"] = st["icur"]

                emitS(0)
                for idx in range(len(units)):
                    emitEM(idx)
                    if idx + 1 < len(units):
                        emitS(idx + 1)
                    emitPV(idx)
                op("dve", lambda e: e.reciprocal(out=accD[:], in_=accD[:]), reads=[B_accD], writes=[B_accD])
                op("dve", lambda e: e.tensor_tensor(out=yb[:], in0=accN[:], in1=accD[:], op=ALU.mult),
                   reads=[B_accN, B_accD], writes=[B_yb])
                dma("sp", lambda e: e.dma_start(out=scrYa[:, 2 * half: 2 * half + 2, QB * 2048:(QB + 1) * 2048], in_=yb[:]),
                    B_yb, reads=[B_yb], writes=[])
                B_scrYa.writes[B_yb.dsem] = B_yb.dcount

        def prefetchA(g):
            j0 = NHIST - HALO + g * 512
            for ti in range(4):
                dma("sp", lambda e: e.dma_start(out=xts[ti][:], in_=xh[j0 + ti * 128: j0 + (ti + 1) * 128, :]),
                    B_xts[ti], writes=[B_xts[ti]])
            for ti in range(4):
                norm_T(xts[ti][:], B_xts[ti], hTb[g % 2][:, :, ti * 128:(ti + 1) * 128], B_hTb[g % 2])

        prefetchA(0)
        for g in range(12):
            u0 = g * 512
            hTc, B_hTc = hTb[g % 2], B_hTb[g % 2]
            kb = u0 // 2048
            for c in range(4):
                ps, B_ps = PS[c % 2], B_PS[c % 2]
                for kc in range(8):
                    op("pe", lambda e: e.matmul(ps[:], lhsT=wA[:, kc, C_KA + c * 128: C_KA + (c + 1) * 128], rhs=hTc[:, kc, :],
                                                start=(kc == 0), stop=(kc == 7)), reads=[B_wA, B_hTc], writes=[B_ps])
                op("act", lambda e: e.activation(out=kT[:, c, u0:u0 + 512], in_=ps[:], func=AF.Copy),
                   reads=[B_ps], writes=[B_kT[kb]])
            if g + 1 < 12:
                prefetchA(g + 1)
            if g >= 4:
                uq0 = ((g - 4) % 4) * 512
                for c in range(4):
                    ps, B_ps = PS[c % 2], B_PS[c % 2]
                    for kc in range(8):
                        op("pe", lambda e: e.matmul(ps[:], lhsT=wA[:, kc, C_QA + c * 128: C_QA + (c + 1) * 128], rhs=hTc[:, kc, :],
                                                    start=(kc == 0), stop=(kc == 7)), reads=[B_wA, B_hTc], writes=[B_ps])
                    op("dve", lambda e: e.tensor_copy(out=qp[0:64, 2 * c, uq0:uq0 + 512], in_=ps[0:64, :]),
                       reads=[B_ps], writes=[B_qp])
                    op("act", lambda e: e.activation(out=qp[64:128, 2 * c + 1, uq0:uq0 + 512], in_=ps[64:128, :], func=AF.Copy),
                       reads=[B_ps], writes=[B_qp])
            for ti in range(4):
                ps, B_ps = PS[ti % 2], B_PS[ti % 2]
                vi = ti % 2
                for kc in range(8):
                    op("pe", lambda e: e.matmul(ps[:], lhsT=hTc[:, kc, ti * 128:(ti + 1) * 128], rhs=wA[:, kc, C_VA:C_VA + 512],
                                                start=(kc == 0), stop=(kc == 7)), reads=[B_wA, B_hTc], writes=[B_ps])
                pv = ps[:].rearrange("p (h e) -> p h e", e=64)
                op("dve", lambda e: e.tensor_copy(out=vpad[vi][:, 0:8:2, 0:64], in_=pv[:, 0:8:2, :]),
                   reads=[B_ps], writes=[B_vpad[vi]])
                op("act", lambda e: e.activation(out=vpad[vi][:, 1:8:2, 64:128], in_=pv[:, 1:8:2, :], func=AF.Copy),
                   reads=[B_ps], writes=[B_vpad[vi]])
                dma("sp", lambda e: e.dma_start(out=scrV[u0 + ti * 128: u0 + (ti + 1) * 128], in_=vpad[vi][:]),
                    B_vpad[vi], reads=[B_vpad[vi]], writes=[B_scrV[kb]])
            if g == 7:
                attention(0)
            if g == 11:
                attention(1)
        f.barrier()
        es.close()
        return B_scrYa

    B_uv = Buf("scrUV")

    def prepass_tables():
        es = ExitStack()
        st = [f.sbuf("pst%d" % i, [128, 8, 1024], F32, es) for i in range(2)]
        B_st = [Buf("pst0"), Buf("pst1")]
        cv = [f.sbuf("pcv%d" % i, [128, 8, 1024], BF16, es) for i in range(2)]
        B_cv = [Buf("pcv0"), Buf("pcv1")]
        dv = scrUV.rearrange("(p r) d -> p r d", p=128)
        jobs = [(tb, c) for tb in range(2) for c in range(16)]

        def load(k):
            tb, c = jobs[k]
            sv = (expert_u if tb == 0 else expert_v).rearrange("(p r) d -> p r d", p=128)
            dma("sp", lambda e: e.dma_start(out=st[k % 2][:], in_=sv[:, 8 * c:8 * c + 8, :]), B_st[k % 2], writes=[B_st[k % 2]])
        load(0)
        for k, (tb, c) in enumerate(jobs):
            i = k % 2
            if k + 1 < len(jobs):
                load(k + 1)
            if k % 2 == 0:
                op("act", lambda e: e.activation(out=cv[i][:], in_=st[i][:], func=AF.Copy), reads=[B_st[i]], writes=[B_cv[i]])
            else:
                op("dve", lambda e: e.tensor_copy(out=cv[i][:], in_=st[i][:]), reads=[B_st[i]], writes=[B_cv[i]])
            dma("sp", lambda e: e.dma_start(out=dv[:, 8 * c:8 * c + 8, tb * 1024:(tb + 1) * 1024], in_=cv[i][:]),
                B_cv[i], reads=[B_cv[i]], writes=[])
            B_uv.writes[B_cv[i].dsem] = B_cv[i].dcount
        f.barrier()
        es.close()

    prepass_tables()

    B_scrYa = pass_attention()

    if stage == 1:
        f.barrier()
        return nc, f


    M_QKM, M_VM, M_OM, M_IF, M_GA, M_GM = 0, 1024, 1536, 2048, 2056, 3080

    def pass_mlstm():
        es = ExitStack()
        wM = f.sbuf("wM", [128, 8, 4104], BF16, es); B_wM = Buf("wM")
        scr4 = f.sbuf("scr4", [128, 1024], F32, es); B_scr4 = Buf("scr4")
        bq_g = f.sbuf("bq_g", [128, 4, 512], F32, es); B_bq = Buf("bq")
        bk_g = f.sbuf("bk_g", [128, 4, 512], F32, es); B_bk = Buf("bk")
        set_stg([bq_g[:, a, :] for a in range(4)] + [bk_g[:, a, :] for a in range(4)])
        load_w(wM, B_wM, w_in, 0, 8, 1536, 4104, gmix, B_gmix)
        wUA = f.sbuf("wUA", [128, 4, 1024], BF16, es); B_wUA = Buf("wUA")
        wUM = f.sbuf("wUM", [128, 4, 1024], BF16, es); B_wUM = Buf("wUM")
        wOut = f.sbuf("wOut", [128, 8, 1024], BF16, es); B_wOut = Buf("wOut")
        load_w(wUA, B_wUA, w_up_attn, 0, 4, 0, 1024)
        load_w(wUM, B_wUM, w_up_mlstm, 0, 4, 0, 1024)
        load_w(wOut, B_wOut, w_out, 0, 8, 0, 1024)
        f.barrier()
        hT2 = f.sbuf("hT2m", [128, 8, 512], BF16, es)
        hTb = [hT, hT2]; B_hTb = [Buf("hTa"), Buf("hTb")]
        mnorm = f.sbuf("mnorm", [128, 512], F32, es); B_mnorm = Buf("mnorm")
        dma("sp", lambda e: e.dma_start(out=mnorm[:], in_=mlstm_norm.to_broadcast([128, 512])), B_mnorm, writes=[B_mnorm])
        gbias = f.sbuf("gbias", [128, 8], F32, es); B_gbias = Buf("gbias")
        dma("sp", lambda e: e.dma_start(out=gbias[:], in_=gate_bias.to_broadcast([128, 8])), B_gbias, writes=[B_gbias])
        cw = f.sbuf("cw", [128, 8, 4], F32, es); B_cw = Buf("cw")
        for k in range(4):
            dma("sp", lambda e: e.dma_start(out=cw[:, :, k], in_=conv_qk[k].rearrange("(c p) -> p c", p=128),
                                            allow_slow_non_contiguous=True), B_cw, writes=[B_cw])
        gif = f.sbuf("gif", [128, 4, 8], F32, es); B_gif = Buf("gif")
        e1 = f.sbuf("e1", [128, 4, 4], F32, es); B_e1 = Buf("e1")
        nsp = f.sbuf("nsp", [128, 4, 4], F32, es); B_nsp = Buf("nsp")
        eFs = f.sbuf("eFs", [128, 4, 4], F32, es); B_eFs = Buf("eFs")
        sp = f.sbuf("spl", [128, 4, 4], F32, es); B_sp = Buf("sp")
        rhsF = scr4[:, 0:512].rearrange("p (a b) -> p a b", a=4); B_rhsF = B_scr4
        rhsK = scr4[:, 512:1024].rearrange("p (a b) -> p a b", a=4); B_rhsK = B_scr4
        tmpI = f.sbuf("tmpI", [128, 4, 128], F32, es); B_tmpI = Buf("tmpI")
        pre = [f.sbuf("pre%d" % i, [128, 515], F32, es) for i in range(2)]
        B_pre = [Buf("pre0"), Buf("pre1")]
        cacc = [f.sbuf("cacc%d" % i, [128, 512], F32, es) for i in range(2)]
        B_cacc = [Buf("cacc0"), Buf("cacc1")]
        carry = f.sbuf("carry", [128, 8, 3], F32, es); B_carry = [Buf("carry%d" % i) for i in range(8)]
        op("pool", lambda e: e.memset(carry[:], 0.0), writes=B_carry)
        qTp = f.sbuf("qTp", [128, 4, 512], BF16, es); B_qTp = Buf("qTp")
        kTp = f.sbuf("kTp", [128, 4, 512], BF16, es); B_kTp = Buf("kTp")
        Vaug = [f.sbuf("Vaug%d" % i, [128, 4, 129], BF16, es) for i in range(2)]
        B_Vaug = [Buf("Vaug0"), Buf("Vaug1")]
        for i in range(2):
            op("pool", lambda e: e.memset(Vaug[i][:], 1.0), writes=[B_Vaug[i]])
        kpt = f.sbuf("kpt", [128, 4, 128], BF16, es); B_kpt = Buf("kpt")
        St = f.sbuf("St", [128, 4, 128], BF16, es); B_St = Buf("St")
        Cs = f.sbuf("Cs", [128, 4, 129], F32, es); B_Cs = Buf("Cs")
        Cb = f.sbuf("Cb", [128, 4, 129], BF16, es); B_Cb = Buf("Cb")
        op("pool", lambda e: e.memset(Cs[:], 0.0), writes=[B_Cs])
        op("pool", lambda e: e.memset(Cb[:], 0.0), writes=[B_Cb])
        dn = f.sbuf("dn", [128, 4], F32, es); B_dn = Buf("dn")
        ssh = f.sbuf("ssh", [128, 4], F32, es); B_ssh = Buf("ssh")
        rn = f.sbuf("rn", [128, 4], F32, es); B_rn = Buf("rn")
        hm = f.sbuf("hm", [128, 4, 128], F32, es); B_hm = Buf("hm")
        sqh = tmpI; B_sqh = B_tmpI
        ymb = f.sbuf("ymb", [128, 512], BF16, es); B_ymb = Buf("ymb")
        ymT = f.sbuf("ymT", [128, 4, 512], BF16, es); B_ymT = Buf("ymT")
        yaT = f.sbuf("yaT", [128, 4, 512], BF16, es); B_yaT = Buf("yaT")
        sigA = f.sbuf("sigA", [128, 512], F32, es); B_sigA = Buf("sigA")
        so = sigA; B_so = B_sigA
        sigM = f.sbuf("sigM", [128, 512], F32, es); B_sigM = Buf("sigM")
        mT = f.sbuf("mT", [128, 8, 512], BF16, es); B_mT = Buf("mT")
        x1t = scr4; B_x1t = B_scr4
        B_scrX1 = Buf("scrX1")
        maskU_bc = maskU[:].unsqueeze(1).to_broadcast([128, 4, 128])
        ident_bc = identf[:].unsqueeze(1).to_broadcast([128, 4, 128])

        def prefetchM(g):
            j0 = g * 512
            for ti in range(4):
                dma("sp", lambda e: e.dma_start(out=xts[ti][:], in_=xh[j0 + ti * 128: j0 + (ti + 1) * 128, :]),
                    B_xts[ti], writes=[B_xts[ti]])
            for ti in range(4):
                norm_T(xts[ti][:], B_xts[ti], hTb[g % 2][:, :, ti * 128:(ti + 1) * 128], B_hTb[g % 2])

        prefetchM(0)
        for g in range(32):
            own = g >= 24
            do_q = g >= 23
            j0 = g * 512
            hTc, B_hTc = hTb[g % 2], B_hTb[g % 2]
            for ti in range(4):
                ps, B_ps = PS[ti % 2], B_PS[ti % 2]
                for kc in range(8):
                    op("pe", lambda e: e.matmul(ps[:, 0:8], lhsT=hTc[:, kc, ti * 128:(ti + 1) * 128], rhs=wM[:, kc, M_IF:M_IF + 8],
                                                start=(kc == 0), stop=(kc == 7)), reads=[B_hTc, B_wM], writes=[B_ps])
                op("dve", lambda e: e.tensor_tensor(out=gif[:, ti, :], in0=ps[:, 0:8], in1=gbias[:], op=ALU.add),
                   reads=[B_ps, B_gbias], writes=[B_gif])
            op("act", lambda e: e.activation(out=e1[:], in_=gif[:, :, 4:8], func=AF.Exp, scale=-1.0),
               reads=[B_gif], writes=[B_e1])
            op("act", lambda e: e.activation(out=sp[:], in_=e1[:], func=AF.Ln, bias=ONE),
               reads=[B_e1, B_cst], writes=[B_sp])
            if not own:
                op("dve", lambda e: e.tensor_scalar(out=nsp[:], in0=sp[:], scalar1=-1.0, scalar2=None, op0=ALU.mult),
                   reads=[B_sp], writes=[B_nsp])
            for ti in range(4):
                sp_bc = sp[:, ti, :].unsqueeze(2).to_broadcast([128, 4, 128])
                i_bc = gif[:, ti, 0:4].unsqueeze(2).to_broadcast([128, 4, 128])
                if own:
                    op("dve", lambda e: e.scalar_tensor_tensor(out=rhsF, in0=maskU_bc, scalar=-1.0, in1=sp_bc,
                                                               op0=ALU.mult, op1=ALU.mult),
                       reads=[B_maskU, B_sp], writes=[B_rhsF])
                op("dve", lambda e: e.tensor_tensor(out=rhsK, in0=maskU_bc, in1=sp_bc, op=ALU.mult),
                   reads=[B_maskU, B_sp], writes=[B_rhsK])
                op("pool", lambda e: e.tensor_tensor(out=tmpI[:], in0=ident_bc, in1=i_bc, op=ALU.mult),
                   reads=[B_identf, B_gif], writes=[B_tmpI])
                op("dve", lambda e: e.tensor_tensor(out=rhsK, in0=rhsK, in1=tmpI[:], op=ALU.add),
                   reads=[B_rhsK, B_tmpI], writes=[B_rhsK])
                if own:
                    op("pe", lambda e: e.matmul(PS[0][:], lhsT=onesf[:], rhs=scr4[:, 0:512], start=True, stop=True),
                       reads=[B_onesf, B_rhsF], writes=[B_PS[0]])
                else:
                    op("pe", lambda e: e.matmul(PS[0][:, 0:4], lhsT=onesf[:], rhs=nsp[:, ti, :], start=True, stop=True),
                       reads=[B_onesf, B_nsp], writes=[B_PS[0]])
                op("pe", lambda e: e.matmul(PS[1][:], lhsT=onesf[:], rhs=scr4[:, 512:1024], start=True, stop=True),
                   reads=[B_onesf, B_rhsK], writes=[B_PS[1]])
                if own:
                    op("act", lambda e: e.activation(out=bq_g[:, :, ti * 128:(ti + 1) * 128],
                                                     in_=PS[0][:].rearrange("p (a b) -> p a b", a=4), func=AF.Exp),
                       reads=[B_PS[0]], writes=[B_bq])
                else:
                    op("act", lambda e: e.activation(out=eFs[:, ti, :], in_=PS[0][:, 0:4], func=AF.Exp),
                       reads=[B_PS[0]], writes=[B_eFs])
                op("act", lambda e: e.activation(out=bk_g[:, :, ti * 128:(ti + 1) * 128],
                                                 in_=PS[1][:].rearrange("p (a b) -> p a b", a=4), func=AF.Exp),
                   reads=[B_PS[1]], writes=[B_bk])
            for c in (range(8) if do_q else range(4, 8)):
                i2 = c % 2
                ps, B_ps = PS[i2], B_PS[i2]
                for kc in range(8):
                    op("pe", lambda e: e.matmul(ps[:], lhsT=wM[:, kc, M_QKM + c * 128: M_QKM + (c + 1) * 128], rhs=hTc[:, kc, :],
                                                start=(kc == 0), stop=(kc == 7)), reads=[B_hTc, B_wM], writes=[B_ps])
                op("pool", lambda e: e.tensor_copy(out=pre[i2][:, 0:3], in_=carry[:, c, :]), reads=[B_carry[c]], writes=[B_pre[i2]])
                op("act", lambda e: e.activation(out=pre[i2][:, 3:515], in_=ps[:], func=AF.Copy), reads=[B_ps], writes=[B_pre[i2]])
                op("pool", lambda e: e.tensor_copy(out=carry[:, c, :], in_=pre[i2][:, 512:515]), reads=[B_pre[i2]], writes=[B_carry[c]])
                op("dve", lambda e: e.tensor_scalar(out=cacc[i2][:], in0=pre[i2][:, 3:515], scalar1=cw[:, c, 3:4], scalar2=None, op0=ALU.mult),
                   reads=[B_pre[i2], B_cw], writes=[B_cacc[i2]])
                for k in (2, 1, 0):
                    op("dve", lambda e: e.scalar_tensor_tensor(out=cacc[i2][:], in0=pre[i2][:, k:k + 512], scalar=cw[:, c, k:k + 1],
                                                               in1=cacc[i2][:], op0=ALU.mult, op1=ALU.add),
                       reads=[B_pre[i2], B_cw, B_cacc[i2]], writes=[B_cacc[i2]])
                op("act", lambda e: e.activation(out=cacc[i2][:], in_=cacc[i2][:], func=AF.Silu), reads=[B_cacc[i2]], writes=[B_cacc[i2]])
                if c < 4:
                    if own:
                        op("dve", lambda e: e.tensor_tensor(out=qTp[:, c, :], in0=cacc[i2][:], in1=bq_g[:, c, :], op=ALU.mult),
                           reads=[B_cacc[i2], B_bq], writes=[B_qTp])
                else:
                    op("dve", lambda e: e.scalar_tensor_tensor(out=kTp[:, c - 4, :], in0=cacc[i2][:], scalar=128.0 ** -0.5,
                                                               in1=bk_g[:, c - 4, :], op0=ALU.mult, op1=ALU.mult),
                       reads=[B_cacc[i2], B_bk], writes=[B_kTp])
            if g + 1 < 32:
                prefetchM(g + 1)
            for ti in range(4):
                tsl = slice(ti * 128, (ti + 1) * 128)
                ps, B_ps = PS[ti % 2], B_PS[ti % 2]
                Va, B_Va = Vaug[ti % 2], B_Vaug[ti % 2]
                for kc in range(8):
                    op("pe", lambda e: e.matmul(ps[:], lhsT=hTc[:, kc, tsl], rhs=wM[:, kc, M_VM:M_VM + 512],
                                                start=(kc == 0), stop=(kc == 7)), reads=[B_hTc, B_wM], writes=[B_ps])
                op("act", lambda e: e.activation(out=Va[:, :, 0:128], in_=ps[:].rearrange("p (a b) -> p a b", a=4), func=AF.Copy),
                   reads=[B_ps], writes=[B_Va])
                for h in range(4):
                    op("pe", lambda e: e.transpose(out=PT[:, h * 128:(h + 1) * 128], in_=kTp[:, h, tsl], identity=identb[:]),
                       reads=[B_kTp, B_identb], writes=[B_PT])
                op("dve", lambda e: e.tensor_copy(out=kpt[:], in_=PT[:, 0:512].rearrange("p (a b) -> p a b", a=4)),
                   reads=[B_PT], writes=[B_kpt])
                if own:
                    for h in range(4):
                        op("pe", lambda e: e.matmul(PS[2][:, h * 128:(h + 1) * 128], lhsT=kTp[:, h, tsl], rhs=qTp[:, h, tsl],
                                                    start=True, stop=True), reads=[B_kTp, B_qTp], writes=[B_PS[2]])
                    op("dve", lambda e: e.tensor_tensor(out=St[:], in0=PS[2][:].rearrange("p (a b) -> p a b", a=4), in1=maskU_bc, op=ALU.mult),
                       reads=[B_PS[2], B_maskU], writes=[B_St])
                    for h in range(4):
                        bank, B_bank = PS[3 + h // 2], B_PS[3 + h // 2]
                        o = (h % 2) * 129
                        op("pe", lambda e: e.matmul(bank[:, o:o + 129], lhsT=St[:, h, :], rhs=Va[:, h, :], start=True, stop=False),
                           reads=[B_St, B_Va], writes=[B_bank])
                        op("pe", lambda e: e.matmul(bank[:, o:o + 129], lhsT=qTp[:, h, tsl], rhs=Cb[:, h, :], start=False, stop=True),
                           reads=[B_qTp, B_Cb], writes=[B_bank])
                    for bi in range(2):
                        op("act", lambda e: e.activation(out=dn[:, 2 * bi:2 * bi + 2],
                                                         in_=PS[3 + bi][:, 0:258].rearrange("p (a c) -> p a c", c=129)[:, :, 128],
                                                         func=AF.Abs),
                           reads=[B_PS[3 + bi]], writes=[B_dn])
                    op("dve", lambda e: e.tensor_scalar_max(out=dn[:], in0=dn[:], scalar1=1.0), reads=[B_dn], writes=[B_dn])
                    op("dve", lambda e: e.reciprocal(out=dn[:], in_=dn[:]), reads=[B_dn], writes=[B_dn])
                    for h in range(4):
                        bank, B_bank = PS[3 + h // 2], B_PS[3 + h // 2]
                        o = (h % 2) * 129
                        op("act", lambda e: e.activation(out=hm[:, h, :], in_=bank[:, o:o + 128], func=AF.Copy, scale=dn[:, h:h + 1]),
                           reads=[B_bank, B_dn], writes=[B_hm])
                    op("pool", lambda e: e.tensor_tensor(out=sqh[:], in0=hm[:], in1=hm[:], op=ALU.mult), reads=[B_hm], writes=[B_sqh])
                    op("dve", lambda e: e.tensor_reduce(out=ssh[:], in_=sqh[:], axis=AX.X, op=ALU.add), reads=[B_sqh], writes=[B_ssh])
                    op("act", lambda e: e.activation(out=rn[:], in_=ssh[:], func=AF.Sqrt, scale=1.0 / 128, bias=EPS),
                       reads=[B_ssh, B_cst], writes=[B_rn])
                    op("dve", lambda e: e.reciprocal(out=rn[:], in_=rn[:]), reads=[B_rn], writes=[B_rn])
                    op("dve", lambda e: e.tensor_tensor(out=hm[:], in0=hm[:], in1=rn[:].unsqueeze(2).to_broadcast([128, 4, 128]), op=ALU.mult),
                       reads=[B_hm, B_rn], writes=[B_hm])
                    hmf = hm[:].rearrange("p a b -> p (a b)")
                    op("pool", lambda e: e.tensor_tensor(out=hmf, in0=hmf, in1=mnorm[:], op=ALU.mult), reads=[B_hm, B_mnorm], writes=[B_hm])
                    for kc in range(8):
                        op("pe", lambda e: e.matmul(ps[:], lhsT=hTc[:, kc, tsl], rhs=wM[:, kc, M_OM:M_OM + 512],
                                                    start=(kc == 0), stop=(kc == 7)), reads=[B_hTc, B_wM], writes=[B_ps])
                    op("act", lambda e: e.activation(out=so[:], in_=ps[:], func=AF.Sigmoid), reads=[B_ps], writes=[B_so])
                    op("dve", lambda e: e.tensor_tensor(out=ymb[:], in0=hmf, in1=so[:], op=ALU.mult), reads=[B_hm, B_so], writes=[B_ymb])
                    for c in range(4):
                        op("pe", lambda e: e.transpose(out=PT[:, 512 + c * 128:512 + (c + 1) * 128], in_=ymb[:, c * 128:(c + 1) * 128],
                                                       identity=identb[:]), reads=[B_ymb, B_identb], writes=[B_PT])
                    op("dve", lambda e: e.tensor_copy(out=ymT[:, :, tsl], in_=PT[:, 512:1024].rearrange("p (a b) -> p a b", a=4)),
                       reads=[B_PT], writes=[B_ymT])
                for h in range(4):
                    bank, B_bank = PS[5 + h // 2], B_PS[5 + h // 2]
                    o = (h % 2) * 129
                    op("pe", lambda e: e.matmul(bank[:, o:o + 129], lhsT=kpt[:, h, :], rhs=Va[:, h, :], start=True, stop=True),
                       reads=[B_kpt, B_Va], writes=[B_bank])
                for bi in range(2):
                    op("dve", lambda e: e.tensor_tensor(out=Cs[:, 2 * bi:2 * bi + 2, :], in0=PS[5 + bi][:, 0:258].rearrange("p (a c) -> p a c", c=129),
                                                        in1=Cs[:, 2 * bi:2 * bi + 2, :], op=ALU.add),
                       reads=[B_PS[5 + bi], B_Cs], writes=[B_Cs])
                if own:
                    eF_bc, B_eF = bq_g[:, :, ti * 128 + 127: ti * 128 + 128].to_broadcast([128, 4, 129]), B_bq
                else:
                    eF_bc, B_eF = eFs[:, ti, :].unsqueeze(2).to_broadcast([128, 4, 129]), B_eFs
                op("dve", lambda e: e.tensor_tensor(out=Cs[:], in0=Cs[:], in1=eF_bc, op=ALU.mult),
                   reads=[B_Cs, B_eF], writes=[B_Cs])
                op("act", lambda e: e.activation(out=Cb[:], in_=Cs[:], func=AF.Copy), reads=[B_Cs], writes=[B_Cb])
            if own:
                tok0 = (g - 24) * 512
                dma("sp", lambda e: e.dma_start(out=yaT[:], in_=scrYa[:, :, tok0:tok0 + 512]), B_yaT, reads=[B_scrYa], writes=[B_yaT])
                for c in range(8):
                    for kc in range(8):
                        op("pe", lambda e: e.matmul(PS[0][:], lhsT=wM[:, kc, M_GA + c * 128: M_GA + (c + 1) * 128], rhs=hTc[:, kc, :],
                                                    start=(kc == 0), stop=(kc == 7)), reads=[B_hTc, B_wM], writes=[B_PS[0]])
                    op("act", lambda e: e.activation(out=sigA[:], in_=PS[0][:], func=AF.Sigmoid), reads=[B_PS[0]], writes=[B_sigA])
                    for kc in range(8):
                        op("pe", lambda e: e.matmul(PS[1][:], lhsT=wM[:, kc, M_GM + c * 128: M_GM + (c + 1) * 128], rhs=hTc[:, kc, :],
                                                    start=(kc == 0), stop=(kc == 7)), reads=[B_hTc, B_wM], writes=[B_PS[1]])
                    op("act", lambda e: e.activation(out=sigM[:], in_=PS[1][:], func=AF.Sigmoid), reads=[B_PS[1]], writes=[B_sigM])
                    for k4 in range(4):
                        op("pe", lambda e: e.matmul(PS[5][:], lhsT=wUA[:, k4, c * 128:(c + 1) * 128], rhs=yaT[:, k4, :],
                                                    start=(k4 == 0), stop=(k4 == 3)), reads=[B_wUA, B_yaT], writes=[B_PS[5]])
                    for k4 in range(4):
                        op("pe", lambda e: e.matmul(PS[6][:], lhsT=wUM[:, k4, c * 128:(c + 1) * 128], rhs=ymT[:, k4, :],
                                                    start=(k4 == 0), stop=(k4 == 3)), reads=[B_wUM, B_ymT], writes=[B_PS[6]])
                    op("dve", lambda e: e.tensor_tensor(out=sigA[:], in0=sigA[:], in1=PS[5][:], op=ALU.mult),
                       reads=[B_sigA, B_PS[5]], writes=[B_sigA])
                    op("dve", lambda e: e.tensor_tensor(out=sigM[:], in0=sigM[:], in1=PS[6][:], op=ALU.mult),
                       reads=[B_sigM, B_PS[6]], writes=[B_sigM])
                    op("pool", lambda e: e.tensor_tensor(out=mT[:, c, :], in0=sigA[:], in1=sigM[:], op=ALU.add),
                       reads=[B_sigA, B_sigM], writes=[B_mT])
                for ti in range(4):
                    tsl = slice(ti * 128, (ti + 1) * 128)
                    jx = j0 + ti * 128
                    dma("sp", lambda e: e.dma_start(out=x1t[:], in_=xh[jx:jx + 128, :]), B_x1t, writes=[B_x1t])
                    for half in range(2):
                        bank, B_bank = PS[3 + half], B_PS[3 + half]
                        for kc in range(8):
                            op("pe", lambda e: e.matmul(bank[:], lhsT=mT[:, kc, tsl], rhs=wOut[:, kc, half * 512:(half + 1) * 512],
                                                        start=(kc == 0), stop=(kc == 7)), reads=[B_mT, B_wOut], writes=[B_bank])
                        op("dve", lambda e: e.tensor_tensor(out=x1t[:, half * 512:(half + 1) * 512], in0=bank[:],
                                                            in1=x1t[:, half * 512:(half + 1) * 512], op=ALU.add),
                           reads=[B_bank, B_x1t], writes=[B_x1t])
                    dma("sp", lambda e: e.dma_start(out=scrX1[tok0 + ti * 128: tok0 + (ti + 1) * 128, :], in_=x1t[:]),
                        B_x1t, reads=[B_x1t], writes=[])
                    B_scrX1.writes[B_x1t.dsem] = B_x1t.dcount
        f.barrier()
        es.close()
        return B_scrX1

    B_scrX1 = pass_mlstm()
    if stage == 2:
        f.barrier()
        return nc, f


    def pass_peer():
        es = ExitStack()
        wQ = f.sbuf("wQ", [128, 8, 2048], BF16, es); B_wQ = Buf("wQ")
        sg = f.sbuf("sg", [128, 1024], F32, es); B_sg = Buf("sg")
        NB = 14
        gb = [f.sbuf("gb%d" % i, [128, 2048], BF16, es) for i in range(NB)]
        B_gb = [Buf("gb%d" % i) for i in range(NB)]
        set_stg([gb[i][:].bitcast(F32)[:, b * 512:(b + 1) * 512] for i in range(4) for b in range(2)])
        stg, B_stg = stg_cur["t"], stg_cur["B"]
        for i in range(2):
            nrm.append(dict(
                ss=f.sbuf("n_ssp%d" % i, [128, 1], F32, es), B_ss=Buf("n_ss"),
                rs=f.sbuf("n_rsp%d" % i, [128, 1], F32, es), B_rs=Buf("n_rs"),
                xs=f.sbuf("n_xsp%d" % i, [128, 1024], BF16, es), B_xs=Buf("n_xs")))
        load_w(wQ, B_wQ, w_query, 0, 8, 0, 2048, gffn, B_gffn)
        wPG = f.sbuf("wPG", [128, 8, 1024], BF16, es); B_wPG = Buf("wPG")
        load_w(wPG, B_wPG, w_ple_gate, 0, 8, 0, 1024, gple, B_gple)
        wPL = f.sbuf("wPL", [128, 2, 1024], BF16, es); B_wPL = Buf("wPL")
        load_w(wPL, B_wPL, w_ple, 0, 2, 0, 1024)
        gffn_bc = f.sbuf("gffn_bc", [128, 1024], F32, es); B_gffn_bc = Buf("gffn_bc")
        gfin_bc = f.sbuf("gfin_bc", [128, 1024], F32, es); B_gfin_bc = Buf("gfin_bc")
        dma("sp", lambda e: e.dma_start(out=gffn_bc[:], in_=norm_ffn.to_broadcast([128, 1024])), B_gffn_bc, writes=[B_gffn_bc])
        dma("sp", lambda e: e.dma_start(out=gfin_bc[:], in_=norm_final.to_broadcast([128, 1024])), B_gfin_bc, writes=[B_gfin_bc])
        keysT = f.sbuf("keysT", [128, 16, 128], BF16, es); B_keysT = Buf("keysT")
        kst = f.sbuf("kst", [128, 128], BF16, es); B_kst = Buf("kst")
        for j in range(16):
            src = (keys1 if j % 2 == 0 else keys2)[j // 2]
            i = stg_i[0] % len(stg); stg_i[0] += 1
            dma("sp", lambda e: e.dma_start(out=stg[i][:, 0:128], in_=src), B_stg[i], writes=[B_stg[i]])
            op("act", lambda e: e.activation(out=kst[:], in_=stg[i][:, 0:128], func=AF.Copy), reads=[B_stg[i]], writes=[B_kst])
            op("pe", lambda e: e.transpose(out=PT[:, 0:128], in_=kst[:], identity=identb[:]), reads=[B_kst, B_identb], writes=[B_PT])
            op("dve", lambda e: e.tensor_copy(out=keysT[:, j, :], in_=PT[:, 0:128]), reads=[B_PT], writes=[B_keysT])
        f.barrier()
        iota16 = f.sbuf("iota16", [128, 16], F32, es); B_iota = Buf("iota16")
        thr16 = f.sbuf("thr16", [128, 16], F32, es); B_thr = Buf("thr16")
        for a in range(16):
            op("pool", lambda e: e.memset(iota16[:, a:a + 1], float(a)), writes=[B_iota])
            op("pool", lambda e: e.memset(thr16[:, a:a + 1], float(16 * a)), writes=[B_thr])
        xn = [f.sbuf("xn%d" % i, [128, 1024], BF16, es) for i in range(2)]; B_xn = [Buf("xn0"), Buf("xn1")]
        qT = f.sbuf("qT", [128, 16, 128], BF16, es); B_qT = Buf("qT")
        sccand = f.sbuf("sccand", [128, 2048], F32, es); B_sc = Buf("sccand")
        sc = sccand.rearrange("p (a b) -> p a b", a=16)
        sc2 = f.sbuf("sc2", [128, 128], F32, es); B_sc2 = Buf("sc2")
        tv = f.sbuf("tv", [128, 16, 16], F32, es); B_tv = Buf("tv")
        tix = f.sbuf("tix", [128, 16, 16], U32, es); B_tix = Buf("tix")
        cand = sccand.rearrange("p (a b c) -> p a b c", a=8, b=16); B_cand = B_sc
        cand2 = f.sbuf("cand2", [128, 256], F32, es); B_cand2 = Buf("cand2")
        ts_ = f.sbuf("ts", [128, 8, 16], F32, es); B_ts = Buf("ts")
        tp = f.sbuf("tp", [128, 8, 16], U32, es); B_tp = Buf("tp")
        tpf = f.sbuf("tpf", [128, 8, 16], F32, es); B_tpf = Buf("tpf")
        negmx = f.sbuf("negmx", [128, 8], F32, es); B_negmx = Buf("negmx")
        ge = [f.sbuf("ge%d" % i, [128, 8, 16], F32, es) for i in range(2)]; B_ge = [Buf("ge0"), Buf("ge1")]
        gs = f.sbuf("gs", [128, 8], F32, es); B_gs = Buf("gs")
        i1f = f.sbuf("i1f", [128, 8, 16], F32, es); B_i1f = Buf("i1f")
        i2f = f.sbuf("i2f", [128, 8, 16], F32, es); B_i2f = Buf("i2f")
        d1 = f.sbuf("d1", [128, 8, 16], F32, es); B_d1 = Buf("d1")
        asum = f.sbuf("asum", [128, 8, 16], F32, es); B_asum = Buf("asum")
        bpos = f.sbuf("bpos", [128, 8, 16], F32, es); B_bpos = Buf("bpos")
        i1s = f.sbuf("i1s", [128, 8, 16], F32, es); B_i1s = Buf("i1s")
        i2s = f.sbuf("i2s", [128, 8, 16], F32, es); B_i2s = Buf("i2s")
        eidf = f.sbuf("eidf", [128, 128], F32, es); B_eidf = Buf("eidf")
        eid = [f.sbuf("eid%d" % i, [128, 128], I32, es) for i in range(2)]; B_eid = [Buf("eid0"), Buf("eid1")]
        apre = f.sbuf("apre", [128, 128], F32, es); B_apre = [Buf("apre%d" % i) for i in range(128)]
        gl = f.sbuf("gl", [128, 128], F32, es); B_gl = [Buf("gl%d" % i) for i in range(128)]
        ga = f.sbuf("ga", [128, 128], F32, es); B_ga = [Buf("ga%d" % i) for i in range(128)]
        junk = f.sbuf("junkp", [128, 1024], BF16, es)
        pt = [f.sbuf("ptile%d" % i, [128, 256], F32, es) for i in range(3)]; B_pt = [Buf("pt0"), Buf("pt1"), Buf("pt2")]
        pb = f.sbuf("pb", [128, 256], BF16, es); B_pb = Buf("pb")
        pT = f.sbuf("pT", [128, 2, 128], BF16, es); B_pT = Buf("pT")
        ot = f.sbuf("ot", [128, 1024], F32, es); B_ot = Buf("ot")
        B_out = Buf("out")
        B_hTs = [Buf("hTs%d" % i) for i in range(4)]
        gcount = [0]

        def top16(src_ap, B_src, tmp, B_tmp, vals, B_vals, idxs, B_idxs):
            op("dve", lambda e: e.max(out=vals[:, 0:8], in_=src_ap), reads=[B_src], writes=[B_vals])
            op("dve", lambda e: e.max_index(out=idxs[:, 0:8], in_max=vals[:, 0:8], in_values=src_ap),
               reads=[B_src, B_vals], writes=[B_idxs])
            op("dve", lambda e: e.match_replace(out=tmp, in_to_replace=vals[:, 0:8], in_values=src_ap, imm_value=-1e30),
               reads=[B_src, B_vals], writes=[B_tmp])
            op("dve", lambda e: e.max(out=vals[:, 8:16], in_=tmp), reads=[B_tmp], writes=[B_vals])
            op("dve", lambda e: e.max_index(out=idxs[:, 8:16], in_max=vals[:, 8:16], in_values=tmp),
               reads=[B_tmp, B_vals], writes=[B_idxs])

        def front(t):
            par = t % 2
            tok0 = t * 128
            x1, B_x1 = xts[t % 4], B_xts[t % 4]
            hs = slice(par * 128, (par + 1) * 128)
            dma("sp", lambda e: e.dma_start(out=x1[:], in_=scrX1[tok0:tok0 + 128, :]), B_x1, reads=[B_scrX1], writes=[B_x1])
            dma("sp", lambda e: e.dma_start(out=pt[t % 3][:], in_=pin[tok0:tok0 + 128, :]), B_pt[t % 3], writes=[B_pt[t % 3]])
            s = yield from norm_T_g(x1[:], B_x1, hT[:, :, hs], B_hTs[par])
            op("dve", lambda e: e.scalar_tensor_tensor(out=xn[par][:], in0=x1[:], scalar=s["rs"][:, 0:1], in1=gffn_bc[:],
                                                       op0=ALU.mult, op1=ALU.mult),
               reads=[B_x1, s["B_rs"], B_gffn_bc], writes=[B_xn[par]])
            yield
            for jb in range(4):
                ps, B_ps = PS[jb % 2], B_PS[jb % 2]
                for jj in range(4):
                    j = 4 * jb + jj
                    for kc in range(8):
                        op("pe", lambda e: e.matmul(ps[:, jj * 128:(jj + 1) * 128], lhsT=wQ[:, kc, j * 128:(j + 1) * 128], rhs=hT[:, kc, hs],
                                                    start=(kc == 0), stop=(kc == 7)), reads=[B_wQ, B_hTs[par]], writes=[B_ps])
                op("act", lambda e: e.activation(out=qT[:, 4 * jb:4 * jb + 4, :], in_=ps[:].rearrange("p (a b) -> p a b", a=4), func=AF.Copy),
                   reads=[B_ps], writes=[B_qT])
                yield
            for jb in range(4):
                ps, B_ps = PS[jb % 2], B_PS[jb % 2]
                for jj in range(4):
                    j = 4 * jb + jj
                    op("pe", lambda e: e.matmul(ps[:, jj * 128:(jj + 1) * 128], lhsT=qT[:, j, :], rhs=keysT[:, j, :], start=True, stop=True),
                       reads=[B_qT, B_keysT], writes=[B_ps])
                op("act", lambda e: e.activation(out=sc[:, 4 * jb:4 * jb + 4, :], in_=ps[:].rearrange("p (a b) -> p a b", a=4), func=AF.Copy),
                   reads=[B_ps], writes=[B_sc])
                yield
            for j in range(16):
                top16(sc[:, j, :], B_sc, sc2[:], B_sc2, tv[:, j, :], B_tv, tix[:, j, :], B_tix)
                yield
            v1 = tv[:, 0:16:2, :].unsqueeze(3).to_broadcast([128, 8, 16, 16])
            v2 = tv[:, 1:16:2, :].unsqueeze(2).to_broadcast([128, 8, 16, 16])
            op("dve", lambda e: e.tensor_tensor(out=cand, in0=v1, in1=v2, op=ALU.add), reads=[B_tv], writes=[B_cand])
            for h in range(8):
                top16(cand[:, h, :, :].rearrange("p a b -> p (a b)"), B_cand, cand2[:], B_cand2, ts_[:, h, :], B_ts, tp[:, h, :], B_tp)
                yield
            op("dve", lambda e: e.tensor_scalar(out=negmx[:], in0=ts_[:, :, 0], scalar1=-1.0, scalar2=None, op0=ALU.mult),
               reads=[B_ts], writes=[B_negmx])
            op("pool", lambda e: e.memset(gs[:], 0.0), writes=[B_gs])
            gep, B_gep = ge[par], B_ge[par]
            for h in range(8):
                op("act", lambda e: e.activation(out=gep[:, h, :], in_=ts_[:, h, :], func=AF.Exp, bias=negmx[:, h:h + 1],
                                                 accum_out=gs[:, h:h + 1]),
                   reads=[B_ts, B_negmx, B_gs], writes=[B_gep, B_gs])
            yield
            op("dve", lambda e: e.reciprocal(out=gs[:], in_=gs[:]), reads=[B_gs], writes=[B_gs])
            op("dve", lambda e: e.tensor_tensor(out=gep[:], in0=gep[:], in1=gs[:].unsqueeze(2).to_broadcast([128, 8, 16]), op=ALU.mult),
               reads=[B_gep, B_gs], writes=[B_gep])
            yield
            op("dve", lambda e: e.tensor_copy(out=tpf[:], in_=tp[:]), reads=[B_tp], writes=[B_tpf])
            op("dve", lambda e: e.tensor_copy(out=i1f[:], in_=tix[:, 0:16:2, :]), reads=[B_tix], writes=[B_i1f])
            op("dve", lambda e: e.tensor_copy(out=i2f[:], in_=tix[:, 1:16:2, :]), reads=[B_tix], writes=[B_i2f])
            op("dve", lambda e: e.tensor_copy(out=d1[:, :, 0:1], in_=i1f[:, :, 0:1]), reads=[B_i1f], writes=[B_d1])
            op("dve", lambda e: e.tensor_tensor(out=d1[:, :, 1:16], in0=i1f[:, :, 1:16], in1=i1f[:, :, 0:15], op=ALU.subtract),
               reads=[B_i1f], writes=[B_d1])
            pos_bc = tpf[:].unsqueeze(3).to_broadcast([128, 8, 16, 16])
            thr_bc = thr16[:].unsqueeze(1).unsqueeze(1).to_broadcast([128, 8, 16, 16])
            iota_bc = iota16[:].unsqueeze(1).unsqueeze(1).to_broadcast([128, 8, 16, 16])
            op("dve", lambda e: e.tensor_tensor(out=cand, in0=pos_bc, in1=thr_bc, op=ALU.is_ge), reads=[B_tpf, B_thr], writes=[B_cand])
            op("dve", lambda e: e.tensor_reduce(out=asum[:], in_=cand, axis=AX.X, op=ALU.add), reads=[B_cand], writes=[B_asum])
            op("dve", lambda e: e.tensor_tensor(out=cand, in0=cand, in1=d1[:].unsqueeze(2).to_broadcast([128, 8, 16, 16]), op=ALU.mult),
               reads=[B_cand, B_d1], writes=[B_cand])
            op("dve", lambda e: e.tensor_reduce(out=i1s[:], in_=cand, axis=AX.X, op=ALU.add), reads=[B_cand], writes=[B_i1s])
            yield
            op("dve", lambda e: e.scalar_tensor_tensor(out=bpos[:], in0=asum[:], scalar=-16.0, in1=tpf[:], op0=ALU.mult, op1=ALU.add),
               reads=[B_asum, B_tpf], writes=[B_bpos])
            op("dve", lambda e: e.tensor_scalar_add(out=bpos[:], in0=bpos[:], scalar1=16.0), reads=[B_bpos], writes=[B_bpos])
            op("dve", lambda e: e.tensor_tensor(out=cand, in0=bpos[:].unsqueeze(3).to_broadcast([128, 8, 16, 16]), in1=iota_bc, op=ALU.is_equal),
               reads=[B_bpos, B_iota], writes=[B_cand])
            op("dve", lambda e: e.tensor_tensor(out=cand, in0=cand, in1=i2f[:].unsqueeze(2).to_broadcast([128, 8, 16, 16]), op=ALU.mult),
               reads=[B_cand, B_i2f], writes=[B_cand])
            op("dve", lambda e: e.tensor_reduce(out=i2s[:], in_=cand, axis=AX.X, op=ALU.add), reads=[B_cand], writes=[B_i2s])
            op("dve", lambda e: e.scalar_tensor_tensor(out=eidf[:], in0=i1s[:].rearrange("p a b -> p (a b)"), scalar=128.0,
                                                       in1=i2s[:].rearrange("p a b -> p (a b)"), op0=ALU.mult, op1=ALU.add),
               reads=[B_i1s, B_i2s], writes=[B_eidf])
            op("dve", lambda e: e.tensor_copy(out=eid[par][:], in_=eidf[:]), reads=[B_eidf], writes=[B_eid[par]])

        dg = [f.sbuf("dg%d" % i, [128, 128], BF16, es) for i in range(4)]
        B_dg = [Buf("dg%d" % i) for i in range(4)]

        def slots(t, bgen=None):
            par = t % 2
            op("pool", lambda e: e.memset(apre[:], 0.0), writes=B_apre)
            gef = ge[par][:].rearrange("p a b -> p (a b)")
            gen = None
            for sl in range(128):
                i = gcount[0] % NB; gcount[0] += 1
                k = sl % 4
                dma("pool", lambda e: e.indirect_dma_start(out=gb[i][:], out_offset=None, in_=scrUV,
                                                           in_offset=bass.IndirectOffsetOnAxis(ap=eid[par][:, sl:sl + 1], axis=0)),
                    B_gb[i], reads=[B_eid[par], B_uv], writes=[B_gb[i]])
                op("dve", lambda e: e.scalar_tensor_tensor(out=junk[:], in0=gb[i][:, 0:1024], scalar=1.0, in1=xn[par][:],
                                                           op0=ALU.mult, op1=ALU.mult, accum_out=apre[:, sl:sl + 1]),
                   reads=[B_gb[i], B_xn[par], B_apre[sl]], writes=[B_apre[sl]], skip_self=True)
                op("act", lambda e: e.activation(out=gl[:, sl:sl + 1], in_=apre[:, sl:sl + 1], func=AF.Gelu),
                   reads=[B_apre[sl]], writes=[B_gl[sl]])
                op("act", lambda e: e.activation(out=ga[:, sl:sl + 1], in_=gl[:, sl:sl + 1], func=AF.Copy, scale=gef[:, sl:sl + 1]),
                   reads=[B_gl[sl], B_ge[par]], writes=[B_ga[sl]])
                op("act", lambda e: e.activation(out=dg[k][:], in_=identf[:], func=AF.Copy, scale=ga[:, sl:sl + 1]),
                   reads=[B_identf, B_ga[sl]], writes=[B_dg[k]])
                for half in range(2):
                    op("pe", lambda e: e.matmul(PS[5 + half][:], lhsT=dg[k][:], rhs=gb[i][:, 1024 + half * 512:1024 + (half + 1) * 512],
                                                start=(sl == 0), stop=(sl == 127)), reads=[B_dg[k], B_gb[i]], writes=[B_PS[5 + half]])
                if sl == 3 and t + 1 < NTOK // 128:
                    gen = front(t + 1)
                if bgen is not None and sl % 2 == 1:
                    try:
                        next(bgen)
                    except StopIteration:
                        bgen = None
                if gen is not None and sl % 3 == 0:
                    try:
                        next(gen)
                    except StopIteration:
                        gen = None
            assert gen is None and bgen is None

        PT2 = PS[4][:].bitcast(BF16)
        B_PT2 = B_PS[4]

        def back(t):
            par = t % 2
            tok0 = t * 128
            x1, B_x1 = xts[t % 4], B_xts[t % 4]
            hs = slice(256 + par * 128, 256 + (par + 1) * 128)
            B_h = B_hTs[2 + par]
            for half in range(2):
                op("dve", lambda e: e.tensor_tensor(out=x1[:, half * 512:(half + 1) * 512], in0=PS[5 + half][:],
                                                    in1=x1[:, half * 512:(half + 1) * 512], op=ALU.add),
                   reads=[B_PS[5 + half], B_x1], writes=[B_x1])
            yield
            yield from norm_T_g(x1[:], B_x1, hT[:, :, hs], B_h, PT2, B_PT2)
            yield
            for half in range(2):
                for kc in range(8):
                    op("pe", lambda e: e.matmul(PS[2 + half][:], lhsT=hT[:, kc, hs], rhs=wPG[:, kc, half * 512:(half + 1) * 512],
                                                start=(kc == 0), stop=(kc == 7)), reads=[B_h, B_wPG], writes=[B_PS[2 + half]])
                op("act", lambda e: e.activation(out=sg[:, half * 512:(half + 1) * 512], in_=PS[2 + half][:], func=AF.Sigmoid),
                   reads=[B_PS[2 + half]], writes=[B_sg])
            op("act", lambda e: e.activation(out=pb[:], in_=pt[t % 3][:], func=AF.Copy), reads=[B_pt[t % 3]], writes=[B_pb])
            for c in range(2):
                op("pe", lambda e: e.transpose(out=PT2[:, c * 128:(c + 1) * 128], in_=pb[:, c * 128:(c + 1) * 128], identity=identb[:]),
                   reads=[B_pb, B_identb], writes=[B_PT2])
            yield
            op("dve", lambda e: e.tensor_copy(out=pT[:], in_=PT2[:, 0:256].rearrange("p (a b) -> p a b", a=2)), reads=[B_PT2], writes=[B_pT])
            for half in range(2):
                for c in range(2):
                    op("pe", lambda e: e.matmul(PS[2 + half][:], lhsT=pT[:, c, :], rhs=wPL[:, c, half * 512:(half + 1) * 512],
                                                start=(c == 0), stop=(c == 1)), reads=[B_pT, B_wPL], writes=[B_PS[2 + half]])
            yield
            for half in range(2):
                hsl = slice(half * 512, (half + 1) * 512)
                op("dve", lambda e: e.tensor_tensor(out=sg[:, hsl], in0=sg[:, hsl], in1=PS[2 + half][:], op=ALU.mult),
                   reads=[B_sg, B_PS[2 + half]], writes=[B_sg])
                op("dve", lambda e: e.tensor_tensor(out=x1[:, hsl], in0=x1[:, hsl], in1=sg[:, hsl], op=ALU.add),
                   reads=[B_x1, B_sg], writes=[B_x1])
            s4 = yield from rms_g(x1[:], B_x1)
            op("dve", lambda e: e.scalar_tensor_tensor(out=ot[:], in0=x1[:], scalar=s4["rs"][:, 0:1], in1=gfin_bc[:],
                                                       op0=ALU.mult, op1=ALU.mult),
               reads=[B_x1, s4["B_rs"], B_gfin_bc], writes=[B_ot])
            dma("sp", lambda e: e.dma_start(out=out_d[tok0:tok0 + 128, :], in_=ot[:]), B_ot, reads=[B_ot], writes=[])
            B_out.writes[B_ot.dsem] = B_ot.dcount

        for _ in front(0):
            pass
        bg = None
        for t in range(NTOK // 128):
            slots(t, bg)
            bg = back(t)
            next(bg)
        for _ in bg:
            pass
        f.barrier()
        es.close()

    pass_peer()
    f.barrier()
    return nc, f


_W_KEYS = ["w_in", "norm_mix", "conv_qk", "gate_bias", "mlstm_norm", "w_up_attn", "w_up_mlstm", "w_out",
           "norm_ffn", "w_query", "keys1", "keys2", "expert_u", "expert_v", "norm_ple", "w_ple_gate", "w_ple"]


def make_in_maps(inputs, cores=range(8)):
    x = np.asarray(inputs["x"], dtype=np.float32)
    p = np.asarray(inputs["p"], dtype=np.float32)[0]
    shared = {}
    for k in _W_KEYS:
        a = np.asarray(inputs[k], dtype=np.float32)[0]
        if k in ("norm_mix", "norm_ple"):
            shared[k] = np.ascontiguousarray(a)
        elif a.ndim == 1:
            shared[k] = np.ascontiguousarray(a[None, :])
        else:
            shared[k] = np.ascontiguousarray(a)
    shared["norm_final"] = np.ascontiguousarray(np.asarray(inputs["norm_final"], dtype=np.float32)[None, :])
    maps = []
    for c in cores:
        b, seg = c // 4, c % 4
        s0 = seg * NTOK
        xhh = np.zeros((NHIST + NTOK, 1024), np.float32)
        if s0 > 0:
            xhh[NHIST - s0:NHIST] = x[b, 0:s0]
        xhh[NHIST:] = x[b, s0:s0 + NTOK]
        m = dict(shared)
        m["xh"] = xhh
        m["p"] = np.ascontiguousarray(p[b, s0:s0 + NTOK])
        m["hv"] = np.full((128, 1), 1.0 if seg > 0 else 0.0, np.float32)
        maps.append(m)
    return maps


def kernel(**inputs):
    nc, f = build()
    maps = make_in_maps(inputs)
    res = run_bass_kernel_spmd(nc, maps, core_ids=list(range(8)))
    out = np.zeros((2, 4 * NTOK, 1024), np.float32)
    for c in range(8):
        b, seg = c // 4, c % 4
        out[b, seg * NTOK:(seg + 1) * NTOK] = res.results[c]["out"]
    return out
```
